# Optimizing a Trainium2 kernel written in Bass

```python
import math
import jax, jax.numpy as jnp
from jax import lax
import numpy as np

D_MODEL = 1024
BATCH = 8
SEQ = 8192
DEPTH = 2

MIX_WIDTH = D_MODEL
HEAD_DIM = 64
ATT_WIDTH = MIX_WIDTH // 2
N_ATT_HEADS = ATT_WIDTH // HEAD_DIM
N_KV_HEADS = 2
ATT_GQA = N_ATT_HEADS // N_KV_HEADS
KV_WIDTH = N_KV_HEADS * HEAD_DIM
N_BRANCH = 3
CMP_BLOCK = 32
CMP_STRIDE = 16
CMP_HIDDEN = 256
SEL_BLOCK = 64
SEL_TOPN = 16
SEL_FORCE_BONUS = 1e4
WINDOW = 512
Q_BLOCK = 64
SSM_WIDTH = MIX_WIDTH - ATT_WIDTH
SSM_HEAD_DIM = 64
N_SSM_HEADS = SSM_WIDTH // SSM_HEAD_DIM
SSM_GROUPS = 2
SSM_STATE = 128
SSM_CONV = 4
SSM_CHUNK = 128
SSM_CONV_DIM = SSM_WIDTH + 2 * SSM_GROUPS * SSM_STATE
FFN_DENSE = 2816
N_EXPERTS = 8
TOP_K = 2
FFN_EXPERT = 3584
MOE_BLOCK = 512
N_DENSE_LAYERS = (DEPTH + 1) // 2
N_MOE_LAYERS = DEPTH // 2
DN_ALPHA = (2 * DEPTH) ** 0.25
DN_BETA = (8 * DEPTH) ** -0.25
LN_EPS = 1e-5
RMS_EPS = 1e-6
NEG = -1e30
IN_SIZES = (ATT_WIDTH, KV_WIDTH, KV_WIDTH, KV_WIDTH, KV_WIDTH, KV_WIDTH, KV_WIDTH, N_ATT_HEADS * N_BRANCH, SSM_WIDTH, SSM_CONV_DIM, N_SSM_HEADS)
IN_WIDTH = sum(IN_SIZES)

kernel_name = 'hybrid_nsa_ssd_moe_deepnorm_block'


def layer_norm(x, w=None, b=None):
    xf = x.astype(jnp.float32)
    mu = jnp.mean(xf, -1, keepdims=True)
    var = jnp.mean(jnp.square(xf - mu), -1, keepdims=True)
    y = (xf - mu) * lax.rsqrt(var + LN_EPS)
    if w is not None:
        y = y * w.astype(jnp.float32) + b.astype(jnp.float32)
    return y.astype(x.dtype)


def rms_norm(x, w):
    xf = x.astype(jnp.float32)
    y = xf * lax.rsqrt(jnp.mean(jnp.square(xf), -1, keepdims=True) + RMS_EPS)
    return (y * w.astype(jnp.float32)).astype(x.dtype)


def masked_softmax(s, mask):
    s = jnp.where(mask, s.astype(jnp.float32), NEG)
    m = jnp.max(s, -1, keepdims=True)
    e = jnp.where(mask, jnp.exp(s - m), 0.0)
    return e / jnp.maximum(jnp.sum(e, -1, keepdims=True), 1e-30)


def compress_blocks(kv, pos, w1, w2):
    b, s, g, d = kv.shape
    n_chunk = s // CMP_STRIDE
    r = CMP_BLOCK // CMP_STRIDE
    n_cmp = n_chunk - r + 1
    ch = kv.reshape(b, n_chunk, CMP_STRIDE, g, d)
    blocks = jnp.concatenate([ch[:, j:j + n_cmp] for j in range(r)], axis=2)
    blocks = blocks + pos[None, None, :, None, :]
    flat = blocks.transpose(0, 1, 3, 2, 4).reshape(b, n_cmp, g, CMP_BLOCK * d)
    return jax.nn.gelu(flat @ w1) @ w2


def nsa_attention(q, kc, vc, ks, vs, kw, vw, gates):
    b, s = q.shape[:2]
    g, r, d = N_KV_HEADS, ATT_GQA, HEAD_DIM
    n_cmp = kc.shape[1]
    n_sel = s // SEL_BLOCK
    top_n = min(SEL_TOPN, n_sel)
    qg = (q * (d ** -0.5)).reshape(b, s, g, r, d)
    gates = gates.reshape(b, s, g, r, N_BRANCH)
    cmp_start = jnp.arange(n_cmp) * CMP_STRIDE
    cmp_last = cmp_start + CMP_BLOCK - 1
    sel_start = jnp.arange(n_sel) * SEL_BLOCK
    overlap = ((cmp_start[:, None] < sel_start[None, :] + SEL_BLOCK) & (cmp_start[:, None] + CMP_BLOCK > sel_start[None, :])).astype(jnp.float32)
    ks_blk = ks.reshape(b, n_sel, SEL_BLOCK, g, d).transpose(0, 3, 1, 2, 4)
    vs_blk = vs.reshape(b, n_sel, SEL_BLOCK, g, d).transpose(0, 3, 1, 2, 4)
    kw_pad = jnp.pad(kw, ((0, 0), (WINDOW, 0), (0, 0), (0, 0)))
    vw_pad = jnp.pad(vw, ((0, 0), (WINDOW, 0), (0, 0), (0, 0)))
    sel_j = jnp.arange(n_sel)
    blk_off = jnp.arange(SEL_BLOCK)
    win_off = jnp.arange(WINDOW + Q_BLOCK)
    gather_blocks = jax.vmap(jax.vmap(lambda kb, ix: kb[ix]))

    def query_block(i):
        s0 = i * Q_BLOCK
        t = s0 + jnp.arange(Q_BLOCK)
        qb = lax.dynamic_slice_in_dim(qg, s0, Q_BLOCK, axis=1)
        gb = lax.dynamic_slice_in_dim(gates, s0, Q_BLOCK, axis=1)
        p_cmp = masked_softmax(jnp.einsum('bqgrd,bcgd->bgrqc', qb, kc), cmp_last[None, :] <= t[:, None])
        o_cmp = jnp.einsum('bgrqc,bcgd->bqgrd', p_cmp.astype(vc.dtype), vc)
        imp = jnp.einsum('bgrqc,cj->bgqj', p_cmp, overlap)
        jt = (t // SEL_BLOCK)[:, None]
        forced = (sel_j == 0) | (sel_j == jt) | (sel_j == jt - 1)
        score = jnp.where(sel_j <= jt, imp + jnp.where(forced, SEL_FORCE_BONUS, 0.0), NEG)
        _, idx = lax.top_k(score, top_n)
        k_sel = gather_blocks(ks_blk, idx).reshape(b, g, Q_BLOCK, top_n * SEL_BLOCK, d)
        v_sel = gather_blocks(vs_blk, idx).reshape(b, g, Q_BLOCK, top_n * SEL_BLOCK, d)
        k_pos = (idx[..., None] * SEL_BLOCK + blk_off).reshape(b, g, Q_BLOCK, top_n * SEL_BLOCK)
        p_sel = masked_softmax(jnp.einsum('bqgrd,bgqkd->bgrqk', qb, k_sel), (k_pos <= t[:, None])[:, :, None])
        o_sel = jnp.einsum('bgrqk,bgqkd->bqgrd', p_sel.astype(v_sel.dtype), v_sel)
        k_win = lax.dynamic_slice_in_dim(kw_pad, s0, WINDOW + Q_BLOCK, axis=1)
        v_win = lax.dynamic_slice_in_dim(vw_pad, s0, WINDOW + Q_BLOCK, axis=1)
        w_pos = s0 - WINDOW + win_off
        w_mask = (w_pos[None, :] <= t[:, None]) & (w_pos[None, :] > t[:, None] - WINDOW) & (w_pos[None, :] >= 0)
        p_win = masked_softmax(jnp.einsum('bqgrd,bkgd->bgrqk', qb, k_win), w_mask)
        o_win = jnp.einsum('bgrqk,bkgd->bqgrd', p_win.astype(v_win.dtype), v_win)
        out = gb[..., 0:1] * o_cmp + gb[..., 1:2] * o_sel + gb[..., 2:3] * o_win
        return out.reshape(b, Q_BLOCK, g * r * d)

    out = lax.map(query_block, jnp.arange(s // Q_BLOCK))
    return out.swapaxes(0, 1).reshape(b, s, g * r * d)


def causal_conv(x, w, bias):
    y = lax.conv_general_dilated(x, w[:, None, :], window_strides=(1,), padding=[(SSM_CONV - 1, 0)],
                                 dimension_numbers=('NWC', 'WIO', 'NWC'), feature_group_count=x.shape[-1])
    return y + bias


def ssd_chunked(x, dt, a, bm, cm, d_skip):
    b, s, h, p = x.shape
    n_chunk = s // SSM_CHUNK
    hpg = h // bm.shape[2]

    def chunks(t):
        return t.astype(jnp.float32).reshape(b, n_chunk, SSM_CHUNK, *t.shape[2:]).swapaxes(0, 1)

    causal = jnp.tril(jnp.ones((SSM_CHUNK, SSM_CHUNK), dtype=bool))[None, :, :, None]

    def step(state, inp):
        xc, dtc, bc, cc = inp
        bh = jnp.repeat(bc, hpg, axis=2)
        chh = jnp.repeat(cc, hpg, axis=2)
        acum = jnp.cumsum(dtc * a, axis=1)
        decay_ij = jnp.exp(jnp.where(causal, acum[:, :, None, :] - acum[:, None, :, :], NEG))
        xdt = xc * dtc[..., None]
        scores = jnp.einsum('bihn,bjhn->bijh', chh, bh) * decay_ij
        y = jnp.einsum('bijh,bjhp->bihp', scores, xdt)
        y = y + jnp.einsum('bihn,bhpn->bihp', chh * jnp.exp(acum)[..., None], state)
        last = acum[:, -1]
        to_end = jnp.exp(last[:, None, :] - acum)
        state = jnp.exp(last)[:, :, None, None] * state + jnp.einsum('bjhn,bjhp->bhpn', bh * to_end[..., None], xdt)
        return state, y + d_skip[:, None] * xc

    state0 = jnp.zeros((b, h, p, bm.shape[-1]), jnp.float32)
    _, ys = lax.scan(step, state0, (chunks(x), chunks(dt), chunks(bm), chunks(cm)))
    return ys.swapaxes(0, 1).reshape(b, s, h * p)


def mixer(u, w_in, cmp_pos_k, cmp_w1_k, cmp_w2_k, cmp_pos_v, cmp_w1_v, cmp_w2_v, attn_norm_w,
          conv_w, conv_b, dt_bias, a_log, d_skip, ssm_norm_w, w_out):
    b, s, _ = u.shape
    offsets = np.cumsum(IN_SIZES)[:-1].tolist()
    q, kc_raw, vc_raw, ks, vs, kw, vw, g_raw, z, xbc, dt_raw = jnp.split(u @ w_in, offsets, axis=-1)
    kvh = lambda t: t.reshape(b, s, N_KV_HEADS, HEAD_DIM)
    kc = compress_blocks(kvh(kc_raw), cmp_pos_k, cmp_w1_k, cmp_w2_k)
    vc = compress_blocks(kvh(vc_raw), cmp_pos_v, cmp_w1_v, cmp_w2_v)
    att = nsa_attention(q.reshape(b, s, N_ATT_HEADS, HEAD_DIM), kc, vc, kvh(ks), kvh(vs), kvh(kw), kvh(vw),
                        jax.nn.sigmoid(g_raw))
    att = rms_norm(att, attn_norm_w)
    xbc = jax.nn.silu(causal_conv(xbc, conv_w, conv_b))
    xs, bm, cm = jnp.split(xbc, [SSM_WIDTH, SSM_WIDTH + SSM_GROUPS * SSM_STATE], axis=-1)
    dt = jax.nn.softplus((dt_raw + dt_bias).astype(jnp.float32))
    a = -jnp.exp(a_log.astype(jnp.float32))
    y = ssd_chunked(xs.reshape(b, s, N_SSM_HEADS, SSM_HEAD_DIM), dt, a,
                    bm.reshape(b, s, SSM_GROUPS, SSM_STATE), cm.reshape(b, s, SSM_GROUPS, SSM_STATE),
                    d_skip.astype(jnp.float32)).astype(u.dtype)
    yz = (y * jax.nn.silu(z)).reshape(b, s, SSM_GROUPS, SSM_WIDTH // SSM_GROUPS)
    ssm = rms_norm(yz, ssm_norm_w.reshape(SSM_GROUPS, -1)).reshape(b, s, SSM_WIDTH)
    return jnp.concatenate([att, ssm], axis=-1) @ w_out


def swiglu(u, wg, wu, wd):
    return (jax.nn.silu(u @ wg) * (u @ wu)) @ wd


def moe_swiglu(u, router_w, wg, wu, wd):
    n_tok, dm = u.shape
    logits = (u @ router_w).astype(jnp.float32)
    top_logit, top_e = lax.top_k(logits, TOP_K)
    gate = jax.nn.softmax(top_logit, axis=-1).astype(u.dtype)
    n_pair = n_tok * TOP_K
    e_flat = top_e.reshape(n_pair)
    tok_flat = jnp.repeat(jnp.arange(n_tok, dtype=jnp.int32), TOP_K)
    order = jnp.argsort(e_flat)
    e_sorted = e_flat[order]
    counts = jnp.bincount(e_flat, length=N_EXPERTS)
    padded = (counts + MOE_BLOCK - 1) // MOE_BLOCK * MOE_BLOCK
    grp_start = jnp.cumsum(counts) - counts
    pad_end = jnp.cumsum(padded)
    pad_start = pad_end - padded
    dest = pad_start[e_sorted] + jnp.arange(n_pair) - grp_start[e_sorted]
    n_blk = -(-n_pair // MOE_BLOCK) + N_EXPERTS
    n_row = n_blk * MOE_BLOCK
    row_tok = jnp.zeros((n_row,), jnp.int32).at[dest].set(tok_flat[order])
    row_gate = jnp.zeros((n_row,), u.dtype).at[dest].set(gate.reshape(n_pair)[order])
    blk_expert = jnp.minimum(jnp.searchsorted(pad_end, jnp.arange(n_blk) * MOE_BLOCK, side='right'), N_EXPERTS - 1)

    def expert_block(args):
        bi, e = args
        xb = u[lax.dynamic_slice_in_dim(row_tok, bi * MOE_BLOCK, MOE_BLOCK)]
        return (jax.nn.silu(xb @ wg[e]) * (xb @ wu[e])) @ wd[e]

    y_rows = lax.map(expert_block, (jnp.arange(n_blk), blk_expert)).reshape(n_row, dm)
    return jnp.zeros_like(u).at[row_tok].add(y_rows * row_gate[:, None])


def setup_inputs(seed: int = 0) -> dict:
    key = jax.random.key(seed)
    keys = iter(jax.random.split(key, 40))
    nrm = lambda shape, scale: jax.random.normal(next(keys), shape, jnp.float32) * scale
    L = DEPTH
    dt0 = jnp.exp(jax.random.uniform(next(keys), (L, N_SSM_HEADS), jnp.float32, math.log(1e-3), math.log(1e-1)))
    return {
        'x': nrm((BATCH, SEQ, D_MODEL), 1.0),
        'c': nrm((BATCH, D_MODEL), 1.0),
        'ada_w': nrm((L, D_MODEL, 6 * D_MODEL), 0.1 * D_MODEL ** -0.5),
        'ada_b': nrm((L, 6 * D_MODEL), 0.01),
        'w_in': nrm((L, D_MODEL, IN_WIDTH), D_MODEL ** -0.5),
        'cmp_pos_k': nrm((L, CMP_BLOCK, HEAD_DIM), 0.02),
        'cmp_w1_k': nrm((L, CMP_BLOCK * HEAD_DIM, CMP_HIDDEN), (CMP_BLOCK * HEAD_DIM) ** -0.5),
        'cmp_w2_k': nrm((L, CMP_HIDDEN, HEAD_DIM), CMP_HIDDEN ** -0.5),
        'cmp_pos_v': nrm((L, CMP_BLOCK, HEAD_DIM), 0.02),
        'cmp_w1_v': nrm((L, CMP_BLOCK * HEAD_DIM, CMP_HIDDEN), (CMP_BLOCK * HEAD_DIM) ** -0.5),
        'cmp_w2_v': nrm((L, CMP_HIDDEN, HEAD_DIM), CMP_HIDDEN ** -0.5),
        'attn_norm_w': 1.0 + nrm((L, ATT_WIDTH), 0.01),
        'conv_w': nrm((L, SSM_CONV, SSM_CONV_DIM), SSM_CONV ** -0.5),
        'conv_b': nrm((L, SSM_CONV_DIM), 0.01),
        'dt_bias': dt0 + jnp.log(-jnp.expm1(-dt0)),
        'a_log': jnp.log(jax.random.uniform(next(keys), (L, N_SSM_HEADS), jnp.float32, 1.0, 16.0)),
        'd_skip': 1.0 + nrm((L, N_SSM_HEADS), 0.01),
        'ssm_norm_w': 1.0 + nrm((L, SSM_WIDTH), 0.01),
        'w_out': nrm((L, MIX_WIDTH, D_MODEL), DN_BETA * MIX_WIDTH ** -0.5),
        'ln1_w': 1.0 + nrm((L, D_MODEL), 0.01),
        'ln1_b': nrm((L, D_MODEL), 0.01),
        'ln2_w': 1.0 + nrm((L, D_MODEL), 0.01),
        'ln2_b': nrm((L, D_MODEL), 0.01),
        'ffn_w_gate': nrm((N_DENSE_LAYERS, D_MODEL, FFN_DENSE), D_MODEL ** -0.5),
        'ffn_w_up': nrm((N_DENSE_LAYERS, D_MODEL, FFN_DENSE), D_MODEL ** -0.5),
        'ffn_w_down': nrm((N_DENSE_LAYERS, FFN_DENSE, D_MODEL), DN_BETA * FFN_DENSE ** -0.5),
        'router_w': nrm((N_MOE_LAYERS, D_MODEL, N_EXPERTS), D_MODEL ** -0.5),
        'exp_w_gate': nrm((N_MOE_LAYERS, N_EXPERTS, D_MODEL, FFN_EXPERT), D_MODEL ** -0.5),
        'exp_w_up': nrm((N_MOE_LAYERS, N_EXPERTS, D_MODEL, FFN_EXPERT), D_MODEL ** -0.5),
        'exp_w_down': nrm((N_MOE_LAYERS, N_EXPERTS, FFN_EXPERT, D_MODEL), DN_BETA * FFN_EXPERT ** -0.5),
    }


def reference(x, c, ada_w, ada_b, w_in, cmp_pos_k, cmp_w1_k, cmp_w2_k, cmp_pos_v, cmp_w1_v, cmp_w2_v,
              attn_norm_w, conv_w, conv_b, dt_bias, a_log, d_skip, ssm_norm_w, w_out,
              ln1_w, ln1_b, ln2_w, ln2_b, ffn_w_gate, ffn_w_up, ffn_w_down,
              router_w, exp_w_gate, exp_w_up, exp_w_down):
    cond = jax.nn.silu(c)
    for layer in range(DEPTH):
        mod = cond @ ada_w[layer] + ada_b[layer]
        shift1, scale1, gate1, shift2, scale2, gate2 = jnp.split(mod[:, None, :], 6, axis=-1)
        u = layer_norm(x) * (1 + scale1) + shift1
        h = mixer(u, w_in[layer], cmp_pos_k[layer], cmp_w1_k[layer], cmp_w2_k[layer],
                  cmp_pos_v[layer], cmp_w1_v[layer], cmp_w2_v[layer], attn_norm_w[layer],
                  conv_w[layer], conv_b[layer], dt_bias[layer], a_log[layer], d_skip[layer],
                  ssm_norm_w[layer], w_out[layer])
        x = layer_norm(DN_ALPHA * x + (1 + gate1) * h, ln1_w[layer], ln1_b[layer])
        u = layer_norm(x) * (1 + scale2) + shift2
        i = layer // 2
        if layer % 2 == 0:
            f = swiglu(u, ffn_w_gate[i], ffn_w_up[i], ffn_w_down[i])
        else:
            f = moe_swiglu(u.reshape(-1, D_MODEL), router_w[i], exp_w_gate[i], exp_w_up[i], exp_w_down[i]).reshape(x.shape)
        x = layer_norm(DN_ALPHA * x + (1 + gate2) * f, ln2_w[layer], ln2_b[layer])
    return x
```

```python
import numpy as np
import ml_dtypes
import concourse.bass as bass
import concourse.mybir as mybir
from concourse.bass_types import AP
from concourse.bass_utils import run_bass_kernel_spmd

F32 = mybir.dt.float32
BF16 = mybir.dt.bfloat16
AF = mybir.ActivationFunctionType
ALU = mybir.AluOpType

S = 8192
D = 1024
NT = S // 128
DEPTH = 2
DN_ALPHA = (2 * DEPTH) ** 0.25
LN_EPS = 1e-5
RMS_EPS = 1e-6
NEGB = -30000.0
W1C = 18 * 128 + 288 + 512
FFN_DENSE = 2816
FFN_EXPERT = 3584
NEXP = 8
EPOCH = 12000


import re
_PSUM_KEY = re.compile(r"^p\d_(ps\d|pT|pa|pb|pf\d|pp|ph|po|S\d|OC|IMP|OS|OW|O2|TP2?|PA|PSc|PD\d|PY\d?|PI|PSt|H\d|PG\d|PU\d|PR)$")


class Em:
    def __init__(self, nc, n_dma_sems=40):
        self.nc = nc
        self.eng = {"pe": nc.tensor, "act": nc.scalar, "dve": nc.vector, "pool": nc.gpsimd, "sp": nc.sync}
        self.cur = {}
        self.nsem = 0
        for e in ("pe", "act", "dve", "pool"):
            self.cur[e] = [self._new_sem(e), 0]
        self.dma_sems = [[self._new_sem("dma"), 0] for _ in range(n_dma_sems)]
        self.dma_rr = 0
        self.seen = {e: {} for e in self.eng}
        self.last_w = {}
        self.readers = {}
        self.ninstr = 0
        self.out_events = []

    def _new_sem(self, tag):
        self.nsem += 1
        return self.nc.alloc_semaphore(f"s_{tag}_{self.nsem}")

    def _wait(self, engname, ev):
        sem, val, src = ev
        sid = id(sem)
        if self.seen[engname].get(sid, 0) >= val:
            return
        self.eng[engname].wait_ge(sem, val)
        self.seen[engname][sid] = val

    def _record(self, ev, reads, writes):
        for k in writes:
            if ev[2] == "dma":
                d = {}
                for e in list(self.last_w.get(k, ())) + [ev]:
                    if e[2] == "dma" and (id(e[0]) not in d or d[id(e[0])][1] < e[1]):
                        d[id(e[0])] = e
                self.last_w[k] = list(d.values())
            else:
                self.last_w[k] = [ev]
            self.readers[k] = []
        for k in reads:
            lst = self.readers.setdefault(k, [])
            lst.append(ev)
            if len(lst) > 8:
                d = {}
                for e in lst:
                    kk = id(e[0])
                    if kk not in d or d[kk][1] < e[1]:
                        d[kk] = e
                self.readers[k] = list(d.values())

    def op(self, engname, fn, reads=(), writes=()):
        for k in reads:
            for ev in self.last_w.get(k, ()):
                self._wait(engname, ev)
            if _PSUM_KEY.match(k):
                for rv in self.readers.get(k, ()):
                    if rv[2] != engname:
                        self._wait(engname, rv)
        for k in writes:
            for ev in self.last_w.get(k, ()):
                if ev[2] != engname or engname != "pe":
                    self._wait(engname, ev)
            for rv in self.readers.get(k, ()):
                if rv[2] != engname:
                    self._wait(engname, rv)
        c = self.cur[engname]
        if c[1] >= EPOCH:
            c[0] = self._new_sem(engname)
            c[1] = 0
        ins = fn()
        ins.then_inc(c[0], 1)
        c[1] += 1
        ev = (c[0], c[1], engname)
        self._record(ev, reads, writes)
        self.ninstr += 1
        return ev

    def dma(self, queue, out, in_, reads=(), writes=(), is_output=False):
        slot = self.dma_sems[self.dma_rr]
        self.dma_rr = (self.dma_rr + 1) % len(self.dma_sems)
        if slot[1] > 0:
            self._wait(queue, (slot[0], slot[1], "dma"))
        for k in reads:
            for ev in self.last_w.get(k, ()):
                self._wait(queue, ev)
        for k in writes:
            for ev in self.last_w.get(k, ()):
                if ev[2] != "dma":
                    self._wait(queue, ev)
            for rv in self.readers.get(k, ()):
                self._wait(queue, rv)
        ins = self.eng[queue].dma_start(out=out, in_=in_)
        slot[1] += 16
        ins.then_inc(slot[0], 16)
        ev = (slot[0], slot[1], "dma")
        self._record(ev, reads, writes)
        self.ninstr += 1
        if is_output:
            self.out_events.append(ev)
        return ev

    def barrier(self):
        for w in ("sp", "act", "pool", "pe", "dve"):
            for slot in self.dma_sems:
                if slot[1] > 0:
                    self._wait(w, (slot[0], slot[1], "dma"))
            for e in ("pe", "act", "dve", "pool"):
                c = self.cur[e]
                if c[1] > 0 and e != w:
                    self._wait(w, (c[0], c[1], e))

    def finish(self):
        for slot in self.dma_sems:
            if slot[1] > 0:
                self._wait("sp", (slot[0], slot[1], "dma"))
        for e in ("pe", "act", "dve", "pool"):
            c = self.cur[e]
            if c[1] > 0:
                self._wait("sp", (c[0], c[1], e))


class Alloc:
    def __init__(self, nc, em=None):
        import contextlib
        self.nc = nc
        self.em = em
        self.st = contextlib.ExitStack()

    def __enter__(self):
        self.st.__enter__()
        return self

    def __exit__(self, *a):
        if self.em is not None and a[0] is None:
            self.em.barrier()
        return self.st.__exit__(*a)

    _uid = [0]

    def sb(self, name, shape, dt):
        Alloc._uid[0] += 1
        return self.st.enter_context(self.nc.sbuf_tensor(f"{name}_{Alloc._uid[0]}", list(shape), dt))

    def ps(self, name, shape, dt):
        Alloc._uid[0] += 1
        return self.st.enter_context(self.nc.psum_tensor(f"{name}_{Alloc._uid[0]}", list(shape), dt))


def bc(ap_, shape, axis):
    return ap_.unsqueeze(axis).to_broadcast(shape)


def row_bc(dram_ap, n):
    return AP(dram_ap.tensor, dram_ap.offset, [[0, 128], [1, n]])


class K:
    def __init__(self, dbg=(), ext_in=(), ext_out=()):
        self.nc = nc = bass.Bass("TRN2", target_bir_lowering=False)
        self.em = Em(nc)
        self.dbg = set(dbg) | set(ext_out)
        self.ext_in = set(ext_in)
        self.I = {}
        self.Sx = {}
        self.qrr = 0

    def inp(self, name, shape, dt=F32):
        t = self.nc.dram_tensor(name, list(shape), dt, kind="ExternalInput").ap()
        return t

    def scratch(self, name, shape, dt):
        kind = "ExternalOutput" if name in self.dbg else ("ExternalInput" if name in self.ext_in else "Internal")
        t = self.nc.dram_tensor(name, list(shape), dt, kind=kind).ap()
        return t

    def q(self):
        self.qrr += 1
        return ("sp", "act")[self.qrr % 2]

    def declare(self):
        T = {}
        def I(name, shape, dt=F32):
            T[name] = (shape, dt)
        I("x", [S, D]); I("cT", [128, 8])
        I("ada_w", [2, D, 6 * D]); I("ada_b", [2, 6 * D])
        I("w_in_r", [2, D, W1C])
        for kv in "kv":
            I(f"cmp_pos_{kv}", [2, 128, 16]); I(f"cmp_w1_{kv}", [2, 2048, 256]); I(f"cmp_w2_{kv}", [2, 256, 64])
        I("attn_norm_w", [2, 512]); I("conv_w_r", [2, 128, 8, 4]); I("conv_b_r", [2, 128, 8])
        I("dt_bias", [2, 8]); I("a_log", [2, 8]); I("d_skip", [2, 8]); I("ssm_norm_w", [2, 512])
        I("w_out", [2, D, D])
        for n in ("ln1_w", "ln1_b", "ln2_w", "ln2_b"):
            I(n, [2, D])
        I("ffn_w_gate", [1, D, FFN_DENSE]); I("ffn_w_up", [1, D, FFN_DENSE]); I("ffn_w_down", [1, FFN_DENSE, D])
        I("router_w", [D, NEXP])
        I("exp_w_gate", [NEXP, D, FFN_EXPERT]); I("exp_w_up", [NEXP, D, FFN_EXPERT]); I("exp_w_down", [NEXP, FFN_EXPERT, D])
        I("ident", [128, 128], BF16); I("E_all", [128, S], BF16); I("overlap", [512, 128], BF16)
        I("cmaskb", [17, 128, 128], BF16); I("cb", [128, 128], BF16); I("wlo", [128, 128], BF16)
        I("bonus", [NT, 128, 128]); I("tri", [128, 128]); I("tris", [128, 128]); I("ones", [128, 128])
        I("tri_bf", [128, 128], BF16); I("ident_f", [128, 128])
        self.itab = T
        ST = {}
        def Sc(name, shape, dt):
            ST[name] = (shape, dt)
        Sc("qT_d", [4, 128, S], BF16); Sc("kselT_d", [128, S], BF16); Sc("kwinT_d", [128, S], BF16)
        Sc("kcr_d", [2, 128, S], BF16); Sc("vcr_d", [2, 128, S], BF16)
        Sc("xbcT_d", [8, 128, S], BF16); Sc("xbc2T_d", [8, 128, S], BF16)
        Sc("vsel_d", [S, 128], BF16); Sc("vwin_d", [S, 128], BF16)
        Sc("gates_d", [S, 24], F32); Sc("z_d", [S, 512], BF16); Sc("dt_d", [S, 8], F32)
        Sc("kcT_d", [128, 512], BF16); Sc("vc_d", [512, 128], BF16)
        Sc("cat_d", [S, D], BF16); Sc("ssm_d", [S, 512], BF16); Sc("att_d", [S, 512], BF16)
        Sc("x1_d", [S, D], F32); Sc("x2_d", [S, D], F32)
        Sc("u2T_d", [8, 128, S], BF16)
        self.stab = ST
        k = self

        class _L(dict):
            def __missing__(d, name):
                if name in T:
                    v = k.inp(name, *T[name])
                else:
                    v = k.scratch(name, *ST[name])
                d[name] = v
                return v
        self.I = _L()
        self.Sx = self.I

    def make_out(self):
        self.out = self.nc.dram_tensor("out", [S, D], F32, kind="ExternalOutput").ap()
        return self.out

    def phase0(self, layer, mod):
        nc, em, I = self.nc, self.em, self.I
        with Alloc(nc, em) as A:
            ct = A.sb("p0_c", [128, 8], F32)
            sg = A.sb("p0_sig", [128, 8], F32)
            cbc = A.sb("p0_cbc", [128, 8, 128], F32)
            w0 = A.sb("p0_w0", [128, 8, 512], F32)
            w1 = A.sb("p0_w1", [128, 8, 512], F32)
            bt = A.sb("p0_b", [128, 6 * D], F32)
            ps0 = A.ps("p0_ps0", [128, 512], F32)
            ps1 = A.ps("p0_ps1", [128, 512], F32)
            em.dma("sp", ct[:], I["cT"], writes=["p0_c"])
            em.dma("act", bt[:], row_bc(I["ada_b"][layer], 6 * D), writes=["p0_b"])
            em.op("act", lambda: nc.scalar.activation(out=sg[:], in_=ct[:], func=AF.Exp, scale=-1.0), reads=["p0_c"], writes=["p0_sig"])
            em.op("dve", lambda: nc.vector.tensor_scalar(out=sg[:], in0=sg[:], scalar1=1.0, scalar2=None, op0=ALU.add), reads=["p0_sig"], writes=["p0_sig"])
            em.op("dve", lambda: nc.vector.reciprocal(out=sg[:], in_=sg[:]), reads=["p0_sig"], writes=["p0_sig"])
            em.op("dve", lambda: nc.vector.tensor_tensor(out=ct[:], in0=ct[:], in1=sg[:], op=ALU.mult), reads=["p0_sig", "p0_c"], writes=["p0_c"])
            em.op("dve", lambda: nc.vector.tensor_copy(out=cbc[:], in_=bc(ct[:], [128, 8, 128], 2)), reads=["p0_c"], writes=["p0_cbc"])
            wb = [(w0, "p0_w0"), (w1, "p0_w1")]
            pss = [(ps0, "p0_ps0"), (ps1, "p0_ps1")]
            for j in range(12):
                wt, wk = wb[j % 2]
                ps, pk = pss[j % 2]
                src = I["ada_w"][layer, :, j * 512:(j + 1) * 512].rearrange("(k p) n -> p k n", p=128)
                em.dma(("sp", "act")[j % 2], wt[:], src, writes=[wk])
                for k in range(8):
                    em.op("pe", lambda k=k, wt=wt, ps=ps: nc.tensor.matmul(ps[:], lhsT=cbc[:, k, :], rhs=wt[:, k, :], start=(k == 0), stop=(k == 7)),
                          reads=[wk, "p0_cbc"], writes=[pk])
                seg = j // 2
                em.op("dve", lambda j=j, ps=ps: nc.vector.tensor_tensor(out=mod[:, j * 512:(j + 1) * 512], in0=ps[:], in1=bt[:, j * 512:(j + 1) * 512], op=ALU.add),
                      reads=[pk, "p0_b"], writes=["mod"])
                if seg in (1, 2, 4, 5):
                    em.op("dve", lambda j=j: nc.vector.tensor_scalar(out=mod[:, j * 512:(j + 1) * 512], in0=mod[:, j * 512:(j + 1) * 512], scalar1=1.0, scalar2=None, op0=ALU.add),
                          reads=["mod"], writes=["mod"])

    def ln_stats(self, xt, xk, st, mv, rstd, key, eps=LN_EPS):
        nc, em = self.nc, self.em
        for h in range(2):
            em.op("dve", lambda h=h: nc.vector.bn_stats(out=st[:, h, :], in_=xt[:, h * 512:(h + 1) * 512]), reads=[xk], writes=[key + "st"])
        em.op("dve", lambda: nc.vector.bn_aggr(out=mv[:], in_=st[:].rearrange("p a b -> p (a b)")), reads=[key + "st"], writes=[key + "mv"])
        em.op("act", lambda: nc.scalar.activation(out=rstd[:], in_=mv[:, 1:2], func=AF.Ln, bias=eps), reads=[key + "mv"], writes=[key + "rs"])
        em.op("act", lambda: nc.scalar.activation(out=rstd[:], in_=rstd[:], func=AF.Exp, scale=-0.5), reads=[key + "rs"], writes=[key + "rs"])
        em.op("dve", lambda: nc.vector.tensor_scalar(out=mv[:, 1:2], in0=mv[:, 0:1], scalar1=rstd[:, 0:1], scalar2=-1.0, op0=ALU.mult, op1=ALU.mult), reads=[key + "mv", key + "rs"], writes=[key + "mv"])

    def phase1(self, layer, x_src, mod, n_super=16):
        nc, em, I, Sx = self.nc, self.em, self.I, self.Sx
        with Alloc(nc, em) as A:
            w = A.sb("p1_w", [128, 8, W1C], BF16)
            ws0 = A.sb("p1_ws0", [128, W1C], F32)
            ws1 = A.sb("p1_ws1", [128, W1C], F32)
            idt = A.sb("p1_id", [128, 128], BF16)
            dtb = A.sb("p1_dtb", [128, 8], F32)
            x0 = A.sb("p1_x0", [128, D], F32)
            x1 = A.sb("p1_x1", [128, D], F32)
            st = A.sb("p1_st", [128, 2, 6], F32)
            mv = A.sb("p1_mv", [128, 2], F32)
            rs = A.sb("p1_rs", [128, 1], F32)
            xn = A.sb("p1_xn", [128, D], F32)
            ub = A.sb("p1_ub", [128, D], BF16)
            uT0 = A.sb("p1_uT0", [128, 8, 512], BF16)
            uT1 = A.sb("p1_uT1", [128, 8, 512], BF16)
            sv = A.sb("p1_sv", [128, 4, 256], BF16)
            sgt = A.sb("p1_sg", [128, 4, 24], F32)
            sdt = A.sb("p1_sd", [128, 4, 8], F32)
            sz = A.sb("p1_sz", [128, 4, 512], BF16)
            f0 = A.sb("p1_f0", [128, 512], BF16)
            f1 = A.sb("p1_f1", [128, 512], BF16)
            f2 = A.sb("p1_f2", [128, 512], BF16)
            f3 = A.sb("p1_f3", [128, 512], BF16)
            pT = A.ps("p1_pT", [128, 1024], BF16)
            pa = A.ps("p1_pa", [128, 512], F32)
            pb = A.ps("p1_pb", [128, 512], F32)
            pf0 = A.ps("p1_pf0", [128, 512], F32)
            pf1 = A.ps("p1_pf1", [128, 512], F32)
            pf2 = A.ps("p1_pf2", [128, 512], F32)
            em.dma("sp", idt[:], I["ident"], writes=["p1_id"])
            em.dma("act", dtb[:], row_bc(I["dt_bias"][layer], 8), writes=["p1_dtb"])
            wss = [(ws0, "p1_ws0"), (ws1, "p1_ws1")]
            for k in range(8):
                wst, wsk = wss[k % 2]
                em.dma(("sp", "act")[k % 2], wst[:], I["w_in_r"][layer, k * 128:(k + 1) * 128, :], writes=[wsk])
                em.op("pool", lambda k=k, wst=wst: nc.gpsimd.tensor_copy(out=w[:, k, :], in_=wst[:]), reads=[wsk], writes=["p1_w"])
            STOP = 99
            xs = [(x0, "p1_x0"), (x1, "p1_x1")]
            uTs = [(uT0, "p1_uT0"), (uT1, "p1_uT1")]
            fst = [(f0, "p1_f0"), (f1, "p1_f1"), (f2, "p1_f2"), (f3, "p1_f3")]
            pfs = [(pf0, "p1_pf0"), (pf1, "p1_pf1"), (pf2, "p1_pf2")]
            fi = 0
            for T in range(n_super):
                uT, uk = uTs[T % 2]
                for sub in range(4):
                    n = T * 4 + sub
                    xt, xk = xs[n % 2]
                    em.dma(("sp", "act")[n % 2], xt[:], x_src[n * 128:(n + 1) * 128, :], writes=[xk])
                    self.ln_stats(xt, xk, st, mv, rs, "p1_")
                    em.op("act", lambda xt=xt: nc.scalar.activation(out=xn[:], in_=xt[:], func=AF.Identity, scale=rs[:, 0:1], bias=mv[:, 1:2]),
                          reads=[xk, "p1_mv", "p1_rs"], writes=["p1_xn"])
                    em.op("dve", lambda: nc.vector.tensor_tensor(out=xn[:], in0=xn[:], in1=mod[:, 1024:2048], op=ALU.mult), reads=["p1_xn", "mod"], writes=["p1_xn"])
                    em.op("pool", lambda: nc.gpsimd.tensor_tensor(out=ub[:], in0=xn[:], in1=mod[:, 0:1024], op=ALU.add), reads=["p1_xn", "mod"], writes=["p1_ub"])
                    if STOP <= 1:
                        continue
                    for k in range(8):
                        em.op("pe", lambda k=k: nc.tensor.transpose(out=pT[:, k * 128:(k + 1) * 128], in_=ub[:, k * 128:(k + 1) * 128], identity=idt[:]),
                              reads=["p1_ub", "p1_id"], writes=["p1_pT"])
                    em.op("act", lambda uT=uT, sub=sub: nc.scalar.copy(out=uT[:, :, sub * 128:(sub + 1) * 128], in_=pT[:].rearrange("p (k t) -> p k t", t=128)),
                          reads=["p1_pT"], writes=[uk])
                    if STOP <= 2:
                        continue
                    for k in range(8):
                        em.op("pe", lambda k=k, uT=uT, sub=sub: nc.tensor.matmul(pa[:, 0:288], lhsT=uT[:, k, sub * 128:(sub + 1) * 128], rhs=w[:, k, 2304:2592], start=(k == 0), stop=(k == 7)),
                              reads=[uk, "p1_w"], writes=["p1_pa"])
                    for k in range(8):
                        em.op("pe", lambda k=k, uT=uT, sub=sub: nc.tensor.matmul(pb[:], lhsT=uT[:, k, sub * 128:(sub + 1) * 128], rhs=w[:, k, 2592:3104], start=(k == 0), stop=(k == 7)),
                              reads=[uk, "p1_w"], writes=["p1_pb"])
                    if STOP <= 3:
                        continue
                    em.op("dve", lambda sub=sub: nc.vector.tensor_copy(out=sv[:, sub, :], in_=pa[:, 0:256]), reads=["p1_pa"], writes=["p1_sv"])
                    em.op("act", lambda sub=sub: nc.scalar.activation(out=sgt[:, sub, :], in_=pa[:, 256:280], func=AF.Exp, scale=-1.0), reads=["p1_pa"], writes=["p1_sg"])
                    em.op("dve", lambda sub=sub: nc.vector.tensor_scalar(out=sgt[:, sub, :], in0=sgt[:, sub, :], scalar1=1.0, scalar2=None, op0=ALU.add), reads=["p1_sg"], writes=["p1_sg"])
                    em.op("dve", lambda sub=sub: nc.vector.reciprocal(out=sgt[:, sub, :], in_=sgt[:, sub, :]), reads=["p1_sg"], writes=["p1_sg"])
                    em.op("dve", lambda sub=sub: nc.vector.tensor_tensor(out=sdt[:, sub, :], in0=pa[:, 280:288], in1=dtb[:], op=ALU.add), reads=["p1_pa", "p1_dtb"], writes=["p1_sd"])
                    em.op("act", lambda sub=sub: nc.scalar.activation(out=sdt[:, sub, :], in_=sdt[:, sub, :], func=AF.Exp), reads=["p1_sd"], writes=["p1_sd"])
                    em.op("act", lambda sub=sub: nc.scalar.activation(out=sdt[:, sub, :], in_=sdt[:, sub, :], func=AF.Ln, bias=1.0), reads=["p1_sd"], writes=["p1_sd"])
                    em.op("act", lambda sub=sub: nc.scalar.copy(out=sz[:, sub, :], in_=pb[:]), reads=["p1_pb"], writes=["p1_sz"])
                if STOP <= 4:
                    continue
                rows = slice(T * 512, (T + 1) * 512)
                em.dma("sp", Sx["vsel_d"][rows, :].rearrange("(s p) c -> p s c", p=128), sv[:, :, 0:128], reads=["p1_sv"], writes=["vsel_d"])
                em.dma("act", Sx["vwin_d"][rows, :].rearrange("(s p) c -> p s c", p=128), sv[:, :, 128:256], reads=["p1_sv"], writes=["vwin_d"])
                em.dma("sp", Sx["gates_d"][rows, :].rearrange("(s p) c -> p s c", p=128), sgt[:], reads=["p1_sg"], writes=["gates_d"])
                em.dma("act", Sx["dt_d"][rows, :].rearrange("(s p) c -> p s c", p=128), sdt[:], reads=["p1_sd"], writes=["dt_d"])
                em.dma("sp", Sx["z_d"][rows, :].rearrange("(s p) c -> p s c", p=128), sz[:], reads=["p1_sz"], writes=["z_d"])
                if STOP <= 5:
                    continue
                cols = slice(T * 512, (T + 1) * 512)
                for ch in range(18):
                    pf, pfk = pfs[ch % 3]
                    fs, fk = fst[fi % 4]
                    fi += 1
                    for k in range(8):
                        em.op("pe", lambda k=k, ch=ch, pf=pf, uT=uT: nc.tensor.matmul(pf[:], lhsT=w[:, k, ch * 128:(ch + 1) * 128], rhs=uT[:, k, :], start=(k == 0), stop=(k == 7)),
                              reads=[uk, "p1_w"], writes=[pfk])
                    if ch < 4:
                        em.op("act", lambda pf=pf, fs=fs: nc.scalar.activation(out=fs[:], in_=pf[:], func=AF.Copy, scale=0.125), reads=[pfk], writes=[fk])
                    elif ch % 2 == 0:
                        em.op("dve", lambda pf=pf, fs=fs: nc.vector.tensor_copy(out=fs[:], in_=pf[:]), reads=[pfk], writes=[fk])
                    else:
                        em.op("act", lambda pf=pf, fs=fs: nc.scalar.copy(out=fs[:], in_=pf[:]), reads=[pfk], writes=[fk])
                    qn = ("sp", "act")[ch % 2]
                    if ch < 4:
                        em.dma(qn, Sx["qT_d"][ch, :, cols], fs[:], reads=[fk], writes=["qT_d"])
                    elif ch == 4:
                        em.dma(qn, Sx["kselT_d"][:, cols], fs[:], reads=[fk], writes=["kselT_d"])
                    elif ch == 5:
                        em.dma(qn, Sx["kwinT_d"][:, cols], fs[:], reads=[fk], writes=["kwinT_d"])
                    elif ch < 10:
                        dst = Sx["kcr_d"] if ch < 8 else Sx["vcr_d"]
                        dk = "kcr_d" if ch < 8 else "vcr_d"
                        g = ch % 2
                        em.dma(qn, dst[g, 0:64, cols], fs[0:64, :], reads=[fk], writes=[dk])
                        if T == 0:
                            em.dma(qn, dst[g, 64:128, 0:511], fs[64:128, 1:512], reads=[fk], writes=[dk])
                        else:
                            em.dma(qn, dst[g, 64:128, T * 512 - 1:T * 512 + 511], fs[64:128, :], reads=[fk], writes=[dk])
                    else:
                        em.dma(qn, Sx["xbcT_d"][ch - 10, :, cols], fs[:], reads=[fk], writes=["xbcT_d"])


    def phase2(self, layer):
        nc, em, I = self.nc, self.em, self.I
        with Alloc(nc, em) as A:
            w1s = A.sb("p2_w1s", [128, 16, 256], F32)
            w1b = A.sb("p2_w1b", [128, 16, 256], BF16)
            poss = A.sb("p2_poss", [128, 16], F32)
            posb = A.sb("p2_posb", [128, 16], BF16)
            w2s = A.sb("p2_w2s", [128, 2, 64], F32)
            w2b = A.sb("p2_w2b", [128, 2, 64], BF16)
            pbias = A.sb("p2_pb", [128, 2], F32)
            kv2 = A.sb("p2_kv2", [128, S], BF16)
            xg = A.sb("p2_xg", [128, 512], F32)
            tg = A.sb("p2_tg", [128, 512], F32)
            g1T = A.sb("p2_g1T", [128, 2, 512], BF16)
            kst = A.sb("p2_kst", [64, 512], BF16)
            vst = A.sb("p2_vst", [128, 4, 64], BF16)
            pp = A.ps("p2_pp", [128, 512], F32)
            ph = A.ps("p2_ph", [128, 512], F32)
            po = A.ps("p2_po", [128, 512], F32)
            em.op("dve", lambda: nc.vector.memset(g1T[:], 0.0), writes=["p2_g1T"])
            em.op("dve", lambda: nc.vector.memset(kst[:], 0.0), writes=["p2_kst"])
            for kv in "kv":
                em.dma("sp", w1s[:], I[f"cmp_w1_{kv}"][layer].rearrange("(m p) h -> p m h", p=128), writes=["p2_w1s"])
                em.dma("act", poss[:], I[f"cmp_pos_{kv}"][layer], writes=["p2_poss"])
                em.dma("act", w2s[:], I[f"cmp_w2_{kv}"][layer].rearrange("(c p) d -> p c d", p=128), writes=["p2_w2s"])
                em.op("pool", lambda: nc.gpsimd.tensor_copy(out=w1b[:], in_=w1s[:]), reads=["p2_w1s"], writes=["p2_w1b"])
                em.op("dve", lambda: nc.vector.tensor_copy(out=posb[:], in_=poss[:]), reads=["p2_poss"], writes=["p2_posb"])
                em.op("dve", lambda: nc.vector.tensor_copy(out=w2b[:], in_=w2s[:]), reads=["p2_w2s"], writes=["p2_w2b"])
                for hc in range(2):
                    for m in range(16):
                        em.op("pe", lambda hc=hc, m=m: nc.tensor.matmul(pp[:, hc:hc + 1], lhsT=w1b[:, m, hc * 128:(hc + 1) * 128], rhs=posb[:, m:m + 1], start=(m == 0), stop=(m == 15)),
                              reads=["p2_w1b", "p2_posb"], writes=["p2_pp"])
                em.op("dve", lambda: nc.vector.tensor_copy(out=pbias[:], in_=pp[:, 0:2]), reads=["p2_pp"], writes=["p2_pb"])
                src = I["kcr_d"] if kv == "k" else I["vcr_d"]
                sk = "kcr_d" if kv == "k" else "vcr_d"
                for g in range(2):
                    for j in range(4):
                        em.dma(("sp", "act")[j % 2], kv2[:, j * 2048:(j + 1) * 2048], src[g, :, j * 2048:(j + 1) * 2048], reads=[sk], writes=["p2_kv2"])
                    base = kv2[:]
                    for hc in range(2):
                        for m in range(16):
                            rhs = AP(base.tensor, base.offset + 2 * m, [[base.ap[0][0], 128], [16, 511]])
                            em.op("pe", lambda hc=hc, m=m, rhs=rhs: nc.tensor.matmul(ph[:, 0:511], lhsT=w1b[:, m, hc * 128:(hc + 1) * 128], rhs=rhs, start=(m == 0), stop=(m == 15)),
                                  reads=["p2_w1b", "p2_kv2"], writes=["p2_ph"])
                        em.op("act", lambda hc=hc: nc.scalar.activation(out=xg[:, 0:511], in_=ph[:, 0:511], func=AF.Identity, bias=pbias[:, hc:hc + 1]),
                              reads=["p2_ph", "p2_pb"], writes=["p2_xg"])
                        em.op("dve", lambda: nc.vector.tensor_tensor(out=tg[:, 0:511], in0=xg[:, 0:511], in1=xg[:, 0:511], op=ALU.mult), reads=["p2_xg"], writes=["p2_tg"])
                        em.op("dve", lambda: nc.vector.tensor_scalar(out=tg[:, 0:511], in0=tg[:, 0:511], scalar1=0.044715, scalar2=1.0, op0=ALU.mult, op1=ALU.add), reads=["p2_tg"], writes=["p2_tg"])
                        em.op("dve", lambda: nc.vector.tensor_tensor(out=tg[:, 0:511], in0=tg[:, 0:511], in1=xg[:, 0:511], op=ALU.mult), reads=["p2_tg", "p2_xg"], writes=["p2_tg"])
                        em.op("act", lambda: nc.scalar.activation(out=tg[:, 0:511], in_=tg[:, 0:511], func=AF.Exp, scale=-1.5957691216057308), reads=["p2_tg"], writes=["p2_tg"])
                        em.op("dve", lambda: nc.vector.tensor_scalar(out=tg[:, 0:511], in0=tg[:, 0:511], scalar1=1.0, scalar2=None, op0=ALU.add), reads=["p2_tg"], writes=["p2_tg"])
                        em.op("dve", lambda: nc.vector.reciprocal(out=tg[:, 0:511], in_=tg[:, 0:511]), reads=["p2_tg"], writes=["p2_tg"])
                        em.op("dve", lambda hc=hc: nc.vector.tensor_tensor(out=g1T[:, hc, 0:511], in0=tg[:, 0:511], in1=xg[:, 0:511], op=ALU.mult), reads=["p2_tg", "p2_xg"], writes=["p2_g1T"])
                    if kv == "k":
                        for hc in range(2):
                            em.op("pe", lambda hc=hc: nc.tensor.matmul(po[0:64, 0:511], lhsT=w2b[:, hc, :], rhs=g1T[:, hc, 0:511], start=(hc == 0), stop=(hc == 1)),
                                  reads=["p2_w2b", "p2_g1T"], writes=["p2_po"])
                        em.op("act", lambda: nc.scalar.copy(out=kst[:, 0:511], in_=po[0:64, 0:511]), reads=["p2_po"], writes=["p2_kst"])
                        em.dma("sp", I["kcT_d"][g * 64:(g + 1) * 64, :], kst[:], reads=["p2_kst"], writes=["kcT_d"])
                    else:
                        for ct in range(4):
                            for hc in range(2):
                                em.op("pe", lambda hc=hc, ct=ct: nc.tensor.matmul(po[:, ct * 64:(ct + 1) * 64], lhsT=g1T[:, hc, ct * 128:(ct + 1) * 128], rhs=w2b[:, hc, :], start=(hc == 0), stop=(hc == 1)),
                                      reads=["p2_w2b", "p2_g1T"], writes=["p2_po"])
                        em.op("act", lambda: nc.scalar.copy(out=vst[:], in_=po[:, 0:256].rearrange("p (c d) -> p c d", d=64)), reads=["p2_po"], writes=["p2_vst"])
                        em.dma("sp", I["vc_d"][:, g * 64:(g + 1) * 64].rearrange("(c p) d -> p c d", p=128), vst[:], reads=["p2_vst"], writes=["vc_d"])

    def phase3(self, layer, n_tiles=NT, n0=0, att_dst=None):
        nc, em, I = self.nc, self.em, self.I
        with Alloc(nc, em) as A:
            kselT = A.sb("p3_kselT", [128, S], BF16)
            kwinT = A.sb("p3_kwinT", [128, S], BF16)
            kcT = A.sb("p3_kcT", [128, 512], BF16)
            vselx = A.sb("p3_vselx", [128, NT, 2, 65], BF16)
            vwinx = A.sb("p3_vwinx", [128, NT, 2, 65], BF16)
            vcx = A.sb("p3_vcx", [128, 4, 2, 65], BF16)
            E = A.sb("p3_E", [128, S], BF16)
            ovl = A.sb("p3_ovl", [128, 4, 128], BF16)
            cmb = A.sb("p3_cmb", [128, 17, 128], BF16)
            idt = A.sb("p3_id", [128, 128], BF16)
            cbt = A.sb("p3_cb", [128, 128], BF16)
            wlot = A.sb("p3_wlo", [128, 128], BF16)
            anw = A.sb("p3_anw", [128, 512], F32)
            qTs = [A.sb(f"p3_qT{i}", [128, 4, 128], BF16) for i in range(2)]
            gats = [A.sb(f"p3_gat{i}", [128, 24], F32) for i in range(2)]
            bons = [A.sb(f"p3_bon{i}", [128, 128], F32) for i in range(2)]
            eTs = [A.sb(f"p3_eT{i}", [128, 512], BF16) for i in range(3)]
            rden = A.sb("p3_rden", [128, 4], F32)
            coef = A.sb("p3_coef", [128, 4], F32)
            score = A.sb("p3_score", [128, 128], F32)
            work = A.sb("p3_work", [128, 128], F32)
            m8 = A.sb("p3_m8", [128, 8], F32)
            m8b = A.sb("p3_m8b", [128, 8], F32)
            self_f = A.sb("p3_self", [128, 128], F32)
            selb = A.sb("p3_selb", [128, 128], BF16)
            selmT = A.sb("p3_selmT", [128, 128], BF16)
            tmp = A.sb("p3_tmp", [128, 4, 64], F32)
            att = A.sb("p3_att", [128, 512], F32)
            junk = A.sb("p3_junk", [128, 512], F32)
            attb = A.sb("p3_attb", [128, 512], BF16)
            ss = A.sb("p3_ss", [128, 1], F32)
            Sb = [A.ps(f"p3_S{i}", [128, 512], F32) for i in range(3)]
            OC = A.ps("p3_OC", [128, 512], F32)
            IMP = A.ps("p3_IMP", [128, 512], F32)
            OS = A.ps("p3_OS", [128, 512], F32)
            OW = A.ps("p3_OW", [128, 512], F32)
            O2 = A.ps("p3_O2", [128, 512], F32)
            TPv = O2[:, 384:512].bitcast(BF16)
            idf = A.sb("p3_idf", [128, 128], F32)
            oTs = A.sb("p3_oTs", [65, 512], F32)
            for j in range(4):
                cs = slice(j * 2048, (j + 1) * 2048)
                em.dma("sp", kselT[:, cs], I["kselT_d"][:, cs], reads=["kselT_d"], writes=["p3_kselT"])
                em.dma("act", kwinT[:, cs], I["kwinT_d"][:, cs], reads=["kwinT_d"], writes=["p3_kwinT"])
                em.dma("sp", E[:, cs], I["E_all"][:, cs], writes=["p3_E"])
            em.dma("act", kcT[:], I["kcT_d"], reads=["kcT_d"], writes=["p3_kcT"])
            for g in range(2):
                for j in range(4):
                    ks = slice(j * 16, (j + 1) * 16)
                    rs_ = slice(j * 2048, (j + 1) * 2048)
                    em.dma("sp", vselx[:, ks, g, 0:64], I["vsel_d"][rs_, g * 64:(g + 1) * 64].rearrange("(k p) d -> p k d", p=128), reads=["vsel_d"], writes=["p3_vselx"])
                    em.dma("act", vwinx[:, ks, g, 0:64], I["vwin_d"][rs_, g * 64:(g + 1) * 64].rearrange("(k p) d -> p k d", p=128), reads=["vwin_d"], writes=["p3_vwinx"])
                em.dma("sp", vcx[:, :, g, 0:64], I["vc_d"][:, g * 64:(g + 1) * 64].rearrange("(k p) d -> p k d", p=128), reads=["vc_d"], writes=["p3_vcx"])
            em.op("dve", lambda: nc.vector.memset(vselx[:, :, :, 64:65], 1.0), writes=["p3_vselx"])
            em.op("dve", lambda: nc.vector.memset(vwinx[:, :, :, 64:65], 1.0), writes=["p3_vwinx"])
            em.op("dve", lambda: nc.vector.memset(vcx[:, :, :, 64:65], 1.0), writes=["p3_vcx"])
            em.dma("sp", ovl[:], I["overlap"].rearrange("(m p) j -> p m j", p=128), writes=["p3_ovl"])
            em.dma("act", cmb[:], I["cmaskb"].rearrange("i p q -> p i q"), writes=["p3_cmb"])
            em.dma("sp", idt[:], I["ident"], writes=["p3_id"])
            em.dma("act", idf[:], I["ident_f"], writes=["p3_idf"])
            em.dma("act", cbt[:], I["cb"], writes=["p3_cb"])
            em.dma("sp", wlot[:], I["wlo"], writes=["p3_wlo"])
            em.dma("act", anw[:], row_bc(I["attn_norm_w"][layer], 512), writes=["p3_anw"])
            st = {"s": 0}

            def nextS():
                i = st["s"] % 3
                st["s"] += 1
                return Sb[i], f"p3_S{i}", eTs[i], f"p3_eT{i}"

            def b4(t):
                return bc(t, [128, 4, 128], 1)

            def s3(ps):
                return ps[:].rearrange("p (r q) -> p r q", r=4)

            def o3(ps, lo, hi):
                return ps[:, 0:260].rearrange("p (r e) -> p r e", e=65)[:, :, lo:hi]

            if att_dst is None:
                att_dst = I["att_d"]
            for n in range(n0, n_tiles):
                qT, qk = qTs[n % 2], f"p3_qT{n % 2}"
                gat, gk = gats[n % 2], f"p3_gat{n % 2}"
                bon, bk = bons[n % 2], f"p3_bon{n % 2}"
                cols = slice(n * 128, (n + 1) * 128)
                em.dma("sp", qT[:], I["qT_d"][:, :, cols].rearrange("r p q -> p r q"), reads=["qT_d"], writes=[qk])
                em.dma("act", gat[:], I["gates_d"][cols, :], reads=["gates_d"], writes=[gk])
                em.dma("act", bon[:], I["bonus"][n], writes=[bk])
                for g in range(2):
                    ps_ = slice(64 * g, 64 * (g + 1))
                    qg = qT[ps_, :, :]
                    n_ct = (8 * n + 6) // 128 + 1
                    for m in range(n_ct):
                        S_, sk, eT, ek = nextS()
                        need = n < 16 * m + 17
                        em.op("pe", lambda S_=S_, m=m, need=need: nc.tensor.matmul(s3(S_), lhsT=kcT[ps_, m * 128:(m + 1) * 128], rhs=qg, start=True, stop=not need),
                              reads=["p3_kcT", qk], writes=[sk])
                        if need:
                            pi = n - 16 * m
                            em.op("pe", lambda S_=S_, pi=pi: nc.tensor.matmul(s3(S_), lhsT=idt[:], rhs=b4(cmb[:, pi, :]), start=False, stop=True),
                                  reads=["p3_id", "p3_cmb"], writes=[sk])
                        em.op("act", lambda S_=S_, eT=eT: nc.scalar.activation(out=eT[:], in_=S_[:], func=AF.Exp), reads=[sk], writes=[ek])
                        for r in range(4):
                            em.op("pe", lambda r=r, m=m, eT=eT: nc.tensor.matmul(OC[:, r * 65:(r + 1) * 65], lhsT=eT[:, r * 128:(r + 1) * 128], rhs=vcx[:, m, g, :],
                                                                               start=(m == 0 and r == 0), stop=(m == n_ct - 1 and r == 3), skip_group_check=True),
                                  reads=[ek, "p3_vcx"], writes=["p3_OC"])
                        for r in range(4):
                            em.op("pe", lambda r=r, m=m, eT=eT: nc.tensor.matmul(IMP[:, r * 128:(r + 1) * 128], lhsT=eT[:, r * 128:(r + 1) * 128], rhs=ovl[:, m, :],
                                                                               start=(m == 0 and r == 0), stop=(m == n_ct - 1 and r == 3), skip_group_check=True),
                                  reads=[ek, "p3_ovl"], writes=["p3_IMP"])
                    em.op("dve", lambda: nc.vector.tensor_scalar(out=rden[:].unsqueeze(2), in0=o3(OC, 64, 65), scalar1=1e-30, scalar2=None, op0=ALU.max), reads=["p3_OC"], writes=["p3_rden"])
                    em.op("dve", lambda: nc.vector.reciprocal(out=rden[:], in_=rden[:]), reads=["p3_rden"], writes=["p3_rden"])
                    em.op("dve", lambda: nc.vector.scalar_tensor_tensor(out=score[:], in0=IMP[:, 0:128], scalar=rden[:, 0:1], in1=bon[:], op0=ALU.mult, op1=ALU.add),
                          reads=["p3_IMP", "p3_rden", bk], writes=["p3_score"])
                    for r in range(1, 4):
                        em.op("dve", lambda r=r: nc.vector.scalar_tensor_tensor(out=score[:], in0=IMP[:, r * 128:(r + 1) * 128], scalar=rden[:, r:r + 1], in1=score[:], op0=ALU.mult, op1=ALU.add),
                              reads=["p3_IMP", "p3_rden", "p3_score"], writes=["p3_score"])
                    gv = gat[:, g * 12:(g + 1) * 12].rearrange("p (r b) -> p r b", b=3)
                    em.op("dve", lambda gv=gv: nc.vector.tensor_tensor(out=coef[:].unsqueeze(2), in0=rden[:].unsqueeze(2), in1=gv[:, :, 0:1], op=ALU.mult), reads=["p3_rden", gk], writes=["p3_coef"])
                    attg = att[:, g * 256:(g + 1) * 256].rearrange("p (r d) -> p r d", d=64)
                    em.op("dve", lambda attg=attg: nc.vector.tensor_tensor(out=attg, in0=o3(OC, 0, 64), in1=bc(coef[:], [128, 4, 64], 2), op=ALU.mult), reads=["p3_OC", "p3_coef"], writes=["p3_att"])
                    em.op("dve", lambda: nc.vector.max(out=m8[:], in_=score[:]), reads=["p3_score"], writes=["p3_m8"])
                    em.op("dve", lambda: nc.vector.match_replace(out=work[:], in_to_replace=m8[:], in_values=score[:], imm_value=-3.0e38), reads=["p3_m8", "p3_score"], writes=["p3_work"])
                    em.op("dve", lambda: nc.vector.max(out=m8b[:], in_=work[:]), reads=["p3_work"], writes=["p3_m8b"])
                    em.op("dve", lambda: nc.vector.tensor_scalar(out=self_f[:], in0=score[:], scalar1=m8b[:, 7:8], scalar2=1.0, op0=ALU.is_ge, op1=ALU.subtract), reads=["p3_score", "p3_m8b"], writes=["p3_self"])
                    em.op("dve", lambda: nc.vector.tensor_scalar(out=selb[:], in0=self_f[:], scalar1=-NEGB, scalar2=None, op0=ALU.mult), reads=["p3_self"], writes=["p3_selb"])
                    em.op("pe", lambda: nc.tensor.transpose(out=TPv[:, 0:128], in_=selb[:], identity=idt[:]), reads=["p3_selb", "p3_id"], writes=["p3_O2"])
                    em.op("act", lambda: nc.scalar.copy(out=selmT[:], in_=TPv[:, 0:128]), reads=["p3_O2"], writes=["p3_selmT"])
                    for (br, kT, kk, vx, vk, O_, ok_, k0) in ((2, kwinT, "p3_kwinT", vwinx, "p3_vwinx", OW, "p3_OW", max(0, n - 4)), (1, kselT, "p3_kselT", vselx, "p3_vselx", OS, "p3_OS", 0)):
                        pend = None

                        def emit_pv(pd, O_=O_, ok_=ok_, vx=vx, vk=vk, k0=k0):
                            kt_, eT_, ek_ = pd
                            em.op("pe", lambda: nc.tensor.matmul(O_[0:65, :], lhsT=vx[:, kt_, g, :], rhs=eT_[:], start=(kt_ == k0), stop=(kt_ == n)),
                                  reads=[ek_, vk], writes=[ok_])

                        for kt in range(k0, n + 1):
                            S_, sk, eT, ek = nextS()
                            extra = []
                            if br == 1:
                                extra.append((E[:, kt * 128:(kt + 1) * 128], "p3_E", selmT, "p3_selmT"))
                            if kt == n:
                                extra.append((idt[:], "p3_id", cbt, "p3_cb"))
                            if br == 2 and kt == n - 4:
                                extra.append((idt[:], "p3_id", wlot, "p3_wlo"))
                            em.op("pe", lambda S_=S_, kt=kt, kT=kT, extra=extra: nc.tensor.matmul(s3(S_), lhsT=kT[ps_, kt * 128:(kt + 1) * 128], rhs=qg, start=True, stop=(len(extra) == 0)),
                                  reads=[kk, qk], writes=[sk])
                            for xi, (lh, lk, rt, rk) in enumerate(extra):
                                em.op("pe", lambda S_=S_, lh=lh, rt=rt, xi=xi, extra=extra: nc.tensor.matmul(s3(S_), lhsT=lh, rhs=b4(rt[:]), start=False, stop=(xi == len(extra) - 1)),
                                      reads=[lk, rk], writes=[sk])
                            em.op("act", lambda S_=S_, eT=eT: nc.scalar.activation(out=eT[:], in_=S_[:], func=AF.Exp), reads=[sk], writes=[ek])
                            if pend is not None:
                                emit_pv(pend)
                            pend = (kt, eT, ek)
                        emit_pv(pend)
                        em.op("act", lambda O_=O_: nc.scalar.copy(out=oTs[:], in_=O_[0:65, :]), reads=[ok_], writes=["p3_oTs"])
                        for r in range(4):
                            em.op("pe", lambda r=r: nc.tensor.transpose(out=O2[:, r * 65:(r + 1) * 65], in_=oTs[0:65, r * 128:(r + 1) * 128], identity=idf[0:65, 0:65]),
                                  reads=["p3_oTs", "p3_idf"], writes=["p3_O2"])
                        O_, ok_ = O2, "p3_O2"
                        em.op("dve", lambda O_=O_: nc.vector.tensor_scalar(out=rden[:].unsqueeze(2), in0=o3(O_, 64, 65), scalar1=1e-30, scalar2=None, op0=ALU.max), reads=[ok_], writes=["p3_rden"])
                        em.op("dve", lambda: nc.vector.reciprocal(out=rden[:], in_=rden[:]), reads=["p3_rden"], writes=["p3_rden"])
                        em.op("dve", lambda gv=gv, br=br: nc.vector.tensor_tensor(out=coef[:].unsqueeze(2), in0=rden[:].unsqueeze(2), in1=gv[:, :, br:br + 1], op=ALU.mult), reads=["p3_rden", gk], writes=["p3_coef"])
                        em.op("dve", lambda O_=O_: nc.vector.tensor_tensor(out=tmp[:], in0=o3(O_, 0, 64), in1=bc(coef[:], [128, 4, 64], 2), op=ALU.mult), reads=[ok_, "p3_coef"], writes=["p3_tmp"])
                        em.op("pool", lambda attg=attg: nc.gpsimd.tensor_tensor(out=attg, in0=attg, in1=tmp[:], op=ALU.add), reads=["p3_tmp", "p3_att"], writes=["p3_att"])
                em.op("act", lambda: nc.scalar.activation(out=junk[:], in_=att[:], func=AF.Square, accum_out=ss[:]), reads=["p3_att"], writes=["p3_junk", "p3_ss"])
                em.op("act", lambda: nc.scalar.activation(out=ss[:], in_=ss[:], func=AF.Ln, scale=1.0 / 512, bias=RMS_EPS), reads=["p3_ss"], writes=["p3_ss"])
                em.op("act", lambda: nc.scalar.activation(out=ss[:], in_=ss[:], func=AF.Exp, scale=-0.5), reads=["p3_ss"], writes=["p3_ss"])
                em.op("dve", lambda: nc.vector.scalar_tensor_tensor(out=attb[:], in0=att[:], scalar=ss[:, 0:1], in1=anw[:], op0=ALU.mult, op1=ALU.mult), reads=["p3_att", "p3_ss", "p3_anw"], writes=["p3_attb"])
                em.dma("sp", att_dst[(n - n0) * 128:(n - n0 + 1) * 128, :], attb[:], reads=["p3_attb"], writes=["att_d"])


    def phase4(self, layer, n_chunks=NT):
        nc, em, I = self.nc, self.em, self.I
        with Alloc(nc, em) as A:
            xin = A.sb("p4_xin", [128, S + 3], BF16)
            cw = A.sb("p4_cw", [128, 8, 4], F32)
            cbs = A.sb("p4_cbs", [128, 8], F32)
            accs = [A.sb(f"p4_acc{i}", [128, 2048], F32) for i in range(2)]
            outs = [A.sb(f"p4_out{i}", [128, 2048], BF16) for i in range(2)]
            em.dma("sp", cw[:], I["conv_w_r"][layer], writes=["p4_cw"])
            em.dma("act", cbs[:], I["conv_b_r"][layer], writes=["p4_cbs"])
            em.op("dve", lambda: nc.vector.memset(xin[:, 0:3], 0.0), writes=["p4_xin"])
            it = 0
            for ch in range(8):
                for j in range(4):
                    em.dma(("sp", "act")[j % 2], xin[:, 3 + j * 2048:3 + (j + 1) * 2048], I["xbcT_d"][ch, :, j * 2048:(j + 1) * 2048], reads=["xbcT_d"], writes=["p4_xin"])
                for j in range(4):
                    acc, ak = accs[it % 2], f"p4_acc{it % 2}"
                    ot, okk = outs[it % 2], f"p4_out{it % 2}"
                    it += 1
                    c0 = j * 2048
                    em.op("dve", lambda acc=acc, c0=c0, ch=ch: nc.vector.tensor_scalar(out=acc[:], in0=xin[:, c0:c0 + 2048], scalar1=cw[:, ch, 0:1], scalar2=cbs[:, ch:ch + 1], op0=ALU.mult, op1=ALU.add),
                          reads=["p4_xin", "p4_cw", "p4_cbs"], writes=[ak])
                    for k in range(1, 4):
                        em.op("dve", lambda acc=acc, c0=c0, ch=ch, k=k: nc.vector.scalar_tensor_tensor(out=acc[:], in0=xin[:, c0 + k:c0 + k + 2048], scalar=cw[:, ch, k:k + 1], in1=acc[:], op0=ALU.mult, op1=ALU.add),
                              reads=["p4_xin", "p4_cw", ak], writes=[ak])
                    em.op("act", lambda acc=acc, ot=ot: nc.scalar.activation(out=ot[:], in_=acc[:], func=AF.Silu), reads=[ak], writes=[okk])
                    em.dma(("sp", "act")[j % 2], I["xbc2T_d"][ch, :, c0:c0 + 2048], ot[:], reads=[okk], writes=["xbc2T_d"])
        with Alloc(nc, em) as A:
            idt = A.sb("p4_id", [128, 128], BF16)
            tri = A.sb("p4_tri", [128, 128], F32)
            tris = A.sb("p4_tris", [128, 128], F32)
            ones = A.sb("p4_ones", [128, 128], F32)
            abc = A.sb("p4_abc", [128, 8], F32)
            dsk = A.sb("p4_dsk", [128, 8], F32)
            snw = A.sb("p4_snw", [128, 512], F32)
            xts = [A.sb(f"p4_xt{i}", [128, 8, 128], BF16) for i in range(2)]
            dts = [A.sb(f"p4_dt{i}", [128, 8], F32) for i in range(2)]
            zts = [A.sb(f"p4_z{i}", [128, 512], BF16) for i in range(2)]
            xb_2 = [A.sb(f"p4_xb{i}", [128, 768], BF16) for i in range(2)]
            dta_2 = [A.sb(f"p4_dta{i}", [128, 8], F32) for i in range(2)]
            sa_2 = [A.sb(f"p4_sa{i}", [128, 16], F32) for i in range(2)]
            eac_2 = [A.sb(f"p4_eac{i}", [128, 8], F32) for i in range(2)]
            te_2 = [A.sb(f"p4_te{i}", [128, 8], F32) for i in range(2)]
            el_2 = [A.sb(f"p4_el{i}", [128, 8], F32) for i in range(2)]
            xdt_2 = [A.sb(f"p4_xdt{i}", [128, 8, 64], BF16) for i in range(2)]
            xdte_2 = [A.sb(f"p4_xdte{i}", [128, 8, 64], BF16) for i in range(2)]
            scm_2 = [A.sb(f"p4_scm{i}", [128, 2, 128], F32) for i in range(2)]
            Rs = [A.sb(f"p4_R{i}", [128, 128], F32) for i in range(2)]
            Wd_2 = [A.sb(f"p4_Wd{i}", [128, 8, 128], F32) for i in range(2)]
            W_2 = [A.sb(f"p4_W{i}", [128, 8, 128], BF16) for i in range(2)]
            stf = A.sb("p4_stf", [128, 8, 64], F32)
            stb = A.sb("p4_stb", [128, 8, 64], BF16)
            ysb_2 = [A.sb(f"p4_ysb{i}", [128, 512], F32) for i in range(2)]
            yi_2 = [A.sb(f"p4_yi{i}", [128, 512], F32) for i in range(2)]
            szt_2 = [A.sb(f"p4_szt{i}", [128, 512], F32) for i in range(2)]
            junk_2 = [A.sb(f"p4_junk{i}", [128, 256], F32) for i in range(2)]
            ss2_2 = [A.sb(f"p4_ss2{i}", [128, 2], F32) for i in range(2)]
            yo_2 = [A.sb(f"p4_yo{i}", [128, 512], BF16) for i in range(2)]
            TP = A.ps("p4_TP", [128, 1024], BF16)
            PA = A.ps("p4_PA", [128, 512], F32)
            PSc = A.ps("p4_PSc", [128, 512], F32)
            PD = [A.ps(f"p4_PD{i}", [128, 512], F32) for i in range(2)]
            PY = A.ps("p4_PY", [128, 512], F32)
            PI = A.ps("p4_PI", [128, 512], F32)
            PSt = A.ps("p4_PSt", [128, 512], F32)
            em.dma("sp", idt[:], I["ident"], writes=["p4_id"])
            em.dma("act", tri[:], I["tri"], writes=["p4_tri"])
            em.dma("sp", tris[:], I["tris"], writes=["p4_tris"])
            em.dma("act", ones[:], I["ones"], writes=["p4_ones"])
            em.dma("sp", abc[:], row_bc(I["a_log"][layer], 8), writes=["p4_abc"])
            em.dma("act", dsk[:], row_bc(I["d_skip"][layer], 8), writes=["p4_dsk"])
            em.dma("sp", snw[:], row_bc(I["ssm_norm_w"][layer], 512), writes=["p4_snw"])
            em.op("act", lambda: nc.scalar.activation(out=abc[:], in_=abc[:], func=AF.Exp), reads=["p4_abc"], writes=["p4_abc"])
            em.op("dve", lambda: nc.vector.tensor_scalar(out=abc[:], in0=abc[:], scalar1=-1.0, scalar2=None, op0=ALU.mult), reads=["p4_abc"], writes=["p4_abc"])
            em.op("dve", lambda: nc.vector.memset(stf[:], 0.0), writes=["p4_stf"])
            em.op("dve", lambda: nc.vector.memset(stb[:], 0.0), writes=["p4_stb"])
            ri = 0
            for c in range(n_chunks):
                p = c % 2
                xb = xb_2[p]
                dta = dta_2[p]
                sa = sa_2[p]
                eac = eac_2[p]
                te = te_2[p]
                el = el_2[p]
                xdt = xdt_2[p]
                xdte = xdte_2[p]
                scm = scm_2[p]
                Wd = Wd_2[p]
                W = W_2[p]
                ysb = ysb_2[p]
                yi = yi_2[p]
                szt = szt_2[p]
                junk = junk_2[p]
                ss2 = ss2_2[p]
                yo = yo_2[p]
                xt, xk = xts[c % 2], f"p4_xt{c % 2}"
                dtt, dk = dts[c % 2], f"p4_dt{c % 2}"
                zt, zk = zts[c % 2], f"p4_z{c % 2}"
                cols = slice(c * 128, (c + 1) * 128)
                em.dma("sp", xt[:], I["xbc2T_d"][:, :, cols].rearrange("c p t -> p c t"), reads=["xbc2T_d"], writes=[xk])
                em.dma("act", dtt[:], I["dt_d"][cols, :], reads=["dt_d"], writes=[dk])
                em.dma("act", zt[:], I["z_d"][cols, :], reads=["z_d"], writes=[zk])
                for k in range(6):
                    em.op("pe", lambda k=k, xt=xt: nc.tensor.transpose(out=TP[:, k * 128:(k + 1) * 128], in_=xt[:, k, :], identity=idt[:]), reads=[xk, "p4_id"], writes=["p4_TP"])
                em.op("act", lambda: nc.scalar.copy(out=xb[:], in_=TP[:, 0:768]), reads=["p4_TP"], writes=[f"p4_xb{p}"])
                em.op("dve", lambda dtt=dtt: nc.vector.tensor_tensor(out=dta[:], in0=dtt[:], in1=abc[:], op=ALU.mult), reads=[dk, "p4_abc"], writes=[f"p4_dta{p}"])
                em.op("pe", lambda: nc.tensor.matmul(PA[:, 0:8], lhsT=tri[:], rhs=dta[:], start=True, stop=True), reads=["p4_tri", f"p4_dta{p}"], writes=["p4_PA"])
                em.op("pe", lambda: nc.tensor.matmul(PA[:, 8:16], lhsT=ones[:], rhs=dta[:], start=True, stop=True), reads=["p4_ones", f"p4_dta{p}"], writes=["p4_PA"])
                em.op("act", lambda: nc.scalar.copy(out=sa[:], in_=PA[:, 0:16]), reads=["p4_PA"], writes=[f"p4_sa{p}"])
                em.op("act", lambda: nc.scalar.activation(out=eac[:], in_=sa[:, 0:8], func=AF.Exp), reads=[f"p4_sa{p}"], writes=[f"p4_eac{p}"])
                em.op("dve", lambda: nc.vector.tensor_tensor(out=te[:], in0=sa[:, 8:16], in1=sa[:, 0:8], op=ALU.subtract), reads=[f"p4_sa{p}"], writes=[f"p4_te{p}"])
                em.op("act", lambda: nc.scalar.activation(out=te[:], in_=te[:], func=AF.Exp), reads=[f"p4_te{p}"], writes=[f"p4_te{p}"])
                em.op("act", lambda: nc.scalar.activation(out=el[:], in_=sa[:, 8:16], func=AF.Exp), reads=[f"p4_sa{p}"], writes=[f"p4_el{p}"])
                xs3 = xb[:, 0:512].rearrange("p (h d) -> p h d", d=64)
                em.op("dve", lambda dtt=dtt, xs3=xs3: nc.vector.tensor_tensor(out=xdt[:], in0=xs3, in1=bc(dtt[:], [128, 8, 64], 2), op=ALU.mult), reads=[f"p4_xb{p}", dk], writes=[f"p4_xdt{p}"])
                em.op("dve", lambda: nc.vector.tensor_tensor(out=xdte[:], in0=xdt[:], in1=bc(te[:], [128, 8, 64], 2), op=ALU.mult), reads=[f"p4_xdt{p}", f"p4_te{p}"], writes=[f"p4_xdte{p}"])
                for g in range(2):
                    em.op("pe", lambda g=g, xt=xt: nc.tensor.matmul(PSc[:, g * 128:(g + 1) * 128], lhsT=xt[:, 4 + g, :], rhs=xt[:, 6 + g, :], start=True, stop=True), reads=[xk], writes=["p4_PSc"])
                em.op("dve", lambda: nc.vector.tensor_tensor(out=scm[:], in0=PSc[:, 0:256].rearrange("p (g i) -> p g i", g=2), in1=bc(tri[:], [128, 2, 128], 1), op=ALU.mult),
                      reads=["p4_PSc", "p4_tri"], writes=[f"p4_scm{p}"])
                for h in range(8):
                    R, rk = Rs[ri % 2], f"p4_R{ri % 2}"
                    ri += 1
                    em.op("pool", lambda R=R, h=h: nc.gpsimd.tensor_scalar(out=R[:], in0=tri[:], scalar1=dta[:, h:h + 1], scalar2=None, op0=ALU.mult), reads=["p4_tri", f"p4_dta{p}"], writes=[rk])
                    em.op("pe", lambda R=R, h=h: nc.tensor.matmul(PD[h // 4][:, (h % 4) * 128:(h % 4 + 1) * 128], lhsT=tris[:], rhs=R[:], start=True, stop=True),
                          reads=["p4_tris", rk], writes=[f"p4_PD{h // 4}"])
                for g in range(2):
                    em.op("act", lambda g=g: nc.scalar.activation(out=Wd[:, g * 4:(g + 1) * 4, :], in_=PD[g][:].rearrange("p (h i) -> p h i", h=4), func=AF.Exp), reads=[f"p4_PD{g}"], writes=[f"p4_Wd{p}"])
                    em.op("dve", lambda g=g: nc.vector.tensor_tensor(out=W[:, g * 4:(g + 1) * 4, :], in0=Wd[:, g * 4:(g + 1) * 4, :], in1=bc(scm[:, g, :], [128, 4, 128], 1), op=ALU.mult),
                          reads=[f"p4_Wd{p}", f"p4_scm{p}"], writes=[f"p4_W{p}"])
                for h in range(8):
                    g = h // 4
                    hs = slice(h * 64, (h + 1) * 64)
                    em.op("pe", lambda h=h, hs=hs: nc.tensor.matmul(PY[:, hs], lhsT=W[:, h, :], rhs=xdt[:, h, :], start=True, stop=True), reads=[f"p4_W{p}", f"p4_xdt{p}"], writes=["p4_PY"])
                    em.op("pe", lambda h=h, hs=hs, g=g, xt=xt: nc.tensor.matmul(PI[:, hs], lhsT=xt[:, 6 + g, :], rhs=stb[:, h, :], start=True, stop=True), reads=[xk, "p4_stb"], writes=["p4_PI"])
                    em.op("pe", lambda h=h, hs=hs, g=g: nc.tensor.matmul(PSt[:, hs], lhsT=xb[:, 512 + g * 128:512 + (g + 1) * 128], rhs=xdte[:, h, :], start=True, stop=True), reads=[f"p4_xb{p}", f"p4_xdte{p}"], writes=["p4_PSt"])
                y3 = lambda t: t[:].rearrange("p (h d) -> p h d", d=64)
                em.op("act", lambda: nc.scalar.copy(out=ysb[:], in_=PY[:]), reads=["p4_PY"], writes=[f"p4_ysb{p}"])
                em.op("dve", lambda: nc.vector.tensor_tensor(out=y3(yi), in0=y3(PI), in1=bc(eac[:], [128, 8, 64], 2), op=ALU.mult), reads=["p4_PI", f"p4_eac{p}"], writes=[f"p4_yi{p}"])
                em.op("pool", lambda: nc.gpsimd.tensor_tensor(out=ysb[:], in0=ysb[:], in1=yi[:], op=ALU.add), reads=[f"p4_ysb{p}", f"p4_yi{p}"], writes=[f"p4_ysb{p}"])
                em.op("dve", lambda xs3=xs3: nc.vector.tensor_tensor(out=y3(yi), in0=xs3, in1=bc(dsk[:], [128, 8, 64], 2), op=ALU.mult), reads=[f"p4_xb{p}", "p4_dsk", f"p4_ysb{p}"], writes=[f"p4_yi{p}"])
                em.op("pool", lambda: nc.gpsimd.tensor_tensor(out=ysb[:], in0=ysb[:], in1=yi[:], op=ALU.add), reads=[f"p4_ysb{p}", f"p4_yi{p}"], writes=[f"p4_ysb{p}"])
                em.op("dve", lambda: nc.vector.tensor_tensor(out=stf[:], in0=stf[:], in1=bc(el[:], [128, 8, 64], 2), op=ALU.mult), reads=["p4_stf", f"p4_el{p}"], writes=["p4_stf"])
                em.op("dve", lambda: nc.vector.tensor_tensor(out=stf[:], in0=stf[:], in1=y3(PSt), op=ALU.add), reads=["p4_stf", "p4_PSt"], writes=["p4_stf"])
                em.op("act", lambda: nc.scalar.copy(out=stb[:], in_=stf[:]), reads=["p4_stf"], writes=["p4_stb"])
                em.op("act", lambda zt=zt: nc.scalar.activation(out=szt[:], in_=zt[:], func=AF.Silu), reads=[zk], writes=[f"p4_szt{p}"])
                em.op("dve", lambda: nc.vector.tensor_tensor(out=ysb[:], in0=ysb[:], in1=szt[:], op=ALU.mult), reads=[f"p4_ysb{p}", f"p4_szt{p}"], writes=[f"p4_ysb{p}"])
                for g in range(2):
                    em.op("act", lambda g=g: nc.scalar.activation(out=junk[:], in_=ysb[:, g * 256:(g + 1) * 256], func=AF.Square, accum_out=ss2[:, g:g + 1]), reads=[f"p4_ysb{p}"], writes=[f"p4_junk{p}", f"p4_ss2{p}"])
                em.op("act", lambda: nc.scalar.activation(out=ss2[:], in_=ss2[:], func=AF.Ln, scale=1.0 / 256, bias=RMS_EPS), reads=[f"p4_ss2{p}"], writes=[f"p4_ss2{p}"])
                em.op("act", lambda: nc.scalar.activation(out=ss2[:], in_=ss2[:], func=AF.Exp, scale=-0.5), reads=[f"p4_ss2{p}"], writes=[f"p4_ss2{p}"])
                for g in range(2):
                    gs = slice(g * 256, (g + 1) * 256)
                    em.op("dve", lambda g=g, gs=gs: nc.vector.scalar_tensor_tensor(out=yo[:, gs], in0=ysb[:, gs], scalar=ss2[:, g:g + 1], in1=snw[:, gs], op0=ALU.mult, op1=ALU.mult),
                          reads=[f"p4_ysb{p}", f"p4_ss2{p}", "p4_snw"], writes=[f"p4_yo{p}"])
                em.dma("sp", I["ssm_d"][cols, :], yo[:], reads=[f"p4_yo{p}"], writes=["ssm_d"])


    def phase5(self, layer, x_src, mod, n_tiles=NT, att_src=None):
        nc, em, I = self.nc, self.em, self.I
        with Alloc(nc, em) as A:
            wo = A.sb("p5_wo", [128, 8, D], BF16)
            wss = [A.sb(f"p5_ws{i}", [128, D], F32) for i in range(2)]
            l1w = A.sb("p5_l1w", [128, D], F32)
            l1b = A.sb("p5_l1b", [128, D], F32)
            idt = A.sb("p5_id", [128, 128], BF16)
            cats = [A.sb(f"p5_cat{i}", [128, D], BF16) for i in range(2)]
            xs = [A.sb(f"p5_x{i}", [128, D], F32) for i in range(2)]
            catT = A.sb("p5_catT", [128, 8, 128], BF16)
            t = A.sb("p5_t", [128, D], F32)
            xn = A.sb("p5_xn", [128, D], F32)
            x1t = A.sb("p5_x1t", [128, D], F32)
            ub = A.sb("p5_ub", [128, D], BF16)
            u2s = A.sb("p5_u2s", [128, 8, 128], BF16)
            st = A.sb("p5_st", [128, 2, 6], F32)
            mv = A.sb("p5_mv", [128, 2], F32)
            rs = A.sb("p5_rs", [128, 1], F32)
            TP = A.ps("p5_TP", [128, 1024], BF16)
            TP2 = A.ps("p5_TP2", [128, 1024], BF16)
            H = [A.ps(f"p5_H{i}", [128, 512], F32) for i in range(2)]
            em.dma("sp", idt[:], I["ident"], writes=["p5_id"])
            em.dma("act", l1w[:], row_bc(I["ln1_w"][layer], D), writes=["p5_l1w"])
            em.dma("sp", l1b[:], row_bc(I["ln1_b"][layer], D), writes=["p5_l1b"])
            for k in range(8):
                em.dma(("sp", "act")[k % 2], wss[k % 2][:], I["w_out"][layer, k * 128:(k + 1) * 128, :], writes=[f"p5_ws{k % 2}"])
                em.op("pool", lambda k=k: nc.gpsimd.tensor_copy(out=wo[:, k, :], in_=wss[k % 2][:]), reads=[f"p5_ws{k % 2}"], writes=["p5_wo"])
            for n in range(n_tiles):
                cat, ck = cats[n % 2], f"p5_cat{n % 2}"
                xt, xk = xs[n % 2], f"p5_x{n % 2}"
                rows = slice(n * 128, (n + 1) * 128)
                a_ap = att_src(n) if att_src is not None else I["att_d"][rows, :]
                em.dma("sp", cat[:, 0:512], a_ap, reads=["att_d"], writes=[ck])
                em.dma("sp", cat[:, 512:1024], I["ssm_d"][rows, :], reads=["ssm_d"], writes=[ck])
                em.dma("act", xt[:], x_src[rows, :], reads=["xsrc"], writes=[xk])
                for k in range(8):
                    em.op("pe", lambda k=k, cat=cat: nc.tensor.transpose(out=TP[:, k * 128:(k + 1) * 128], in_=cat[:, k * 128:(k + 1) * 128], identity=idt[:]), reads=[ck, "p5_id"], writes=["p5_TP"])
                em.op("act", lambda: nc.scalar.copy(out=catT[:], in_=TP[:].rearrange("p (k t) -> p k t", t=128)), reads=["p5_TP"], writes=["p5_catT"])
                for hf in range(2):
                    for k in range(8):
                        em.op("pe", lambda k=k, hf=hf: nc.tensor.matmul(H[hf][:], lhsT=catT[:, k, :], rhs=wo[:, k, hf * 512:(hf + 1) * 512], start=(k == 0), stop=(k == 7)),
                              reads=["p5_catT", "p5_wo"], writes=[f"p5_H{hf}"])
                    em.op("dve", lambda hf=hf: nc.vector.tensor_tensor(out=t[:, hf * 512:(hf + 1) * 512], in0=H[hf][:], in1=mod[:, 2048 + hf * 512:2048 + (hf + 1) * 512], op=ALU.mult),
                          reads=[f"p5_H{hf}", "mod"], writes=["p5_t"])
                em.op("dve", lambda xt=xt: nc.vector.scalar_tensor_tensor(out=t[:], in0=xt[:], scalar=DN_ALPHA, in1=t[:], op0=ALU.mult, op1=ALU.add), reads=[xk, "p5_t"], writes=["p5_t"])
                self.ln_stats(t, "p5_t", st, mv, rs, "p5_")
                em.op("act", lambda: nc.scalar.activation(out=xn[:], in_=t[:], func=AF.Identity, scale=rs[:, 0:1], bias=mv[:, 1:2]), reads=["p5_t", "p5_mv", "p5_rs"], writes=["p5_xn"])
                em.op("dve", lambda: nc.vector.tensor_tensor(out=xn[:], in0=xn[:], in1=l1w[:], op=ALU.mult), reads=["p5_xn", "p5_l1w"], writes=["p5_xn"])
                em.op("pool", lambda: nc.gpsimd.tensor_tensor(out=x1t[:], in0=xn[:], in1=l1b[:], op=ALU.add), reads=["p5_xn", "p5_l1b"], writes=["p5_x1t"])
                em.dma("sp", I["x1_d"][rows, :], x1t[:], reads=["p5_x1t"], writes=["x1_d"])
                self.ln_stats(x1t, "p5_x1t", st, mv, rs, "p5_")
                em.op("act", lambda: nc.scalar.activation(out=xn[:], in_=x1t[:], func=AF.Identity, scale=rs[:, 0:1], bias=mv[:, 1:2]), reads=["p5_x1t", "p5_mv", "p5_rs"], writes=["p5_xn"])
                em.op("dve", lambda: nc.vector.tensor_tensor(out=xn[:], in0=xn[:], in1=mod[:, 4096:5120], op=ALU.mult), reads=["p5_xn", "mod"], writes=["p5_xn"])
                em.op("pool", lambda: nc.gpsimd.tensor_tensor(out=ub[:], in0=xn[:], in1=mod[:, 3072:4096], op=ALU.add), reads=["p5_xn", "mod"], writes=["p5_ub"])
                for k in range(8):
                    em.op("pe", lambda k=k: nc.tensor.transpose(out=TP2[:, k * 128:(k + 1) * 128], in_=ub[:, k * 128:(k + 1) * 128], identity=idt[:]), reads=["p5_ub", "p5_id"], writes=["p5_TP2"])
                em.op("act", lambda: nc.scalar.copy(out=u2s[:], in_=TP2[:].rearrange("p (k t) -> p k t", t=128)), reads=["p5_TP2"], writes=["p5_u2s"])
                em.dma("act", I["u2T_d"][:, :, rows].rearrange("c p t -> p c t"), u2s[:], reads=["p5_u2s"], writes=["u2T_d"])

    def phase6(self, layer, mod, moe, dst, n_tt=8, is_output=False, t0=0):
        nc, em, I = self.nc, self.em, self.I
        NE = NEXP if moe else 1
        F = FFN_EXPERT if moe else FFN_DENSE
        NFG = F // 256
        with Alloc(nc, em) as A:
            u2T = A.sb("p6_u2T", [128, 8, 1024], BF16)
            acc = A.sb("p6_ACC", [128, 8, D], F32)
            wgs = [A.sb(f"p6_wgs{i}", [128, 8, 256], F32) for i in range(2)]
            wus = [A.sb(f"p6_wus{i}", [128, 8, 256], F32) for i in range(2)]
            wds = [A.sb(f"p6_wds{i}", [128, 2, D], F32) for i in range(2)]
            wgb = [A.sb(f"p6_wgb{i}", [128, 8, 256], BF16) for i in range(2)]
            wub = [A.sb(f"p6_wub{i}", [128, 8, 256], BF16) for i in range(2)]
            wdb = [A.sb(f"p6_wdb{i}", [128, 2, D], BF16) for i in range(2)]
            hTs = [A.sb(f"p6_hT{i}", [128, 2, 1024], BF16) for i in range(2)]
            sgs = [A.sb(f"p6_sg{i}", [128, 512], F32) for i in range(2)]
            l2w = A.sb("p6_l2w", [128, D], F32)
            l2b = A.sb("p6_l2b", [128, D], F32)
            x1s = [A.sb(f"p6_x1{i}", [128, D], F32) for i in range(2)]
            t = A.sb("p6_t", [128, D], F32)
            xn = A.sb("p6_xn", [128, D], F32)
            ot = A.sb("p6_ot", [128, D], F32)
            st = A.sb("p6_st", [128, 2, 6], F32)
            mv = A.sb("p6_mv", [128, 2], F32)
            rs = A.sb("p6_rs", [128, 1], F32)
            G = A.sb("p6_G", [128, 8, 8], F32)
            rws = A.sb("p6_rws", [128, 8, 8], F32)
            rwb = A.sb("p6_rwb", [128, 8, 8], BF16)
            lg = A.sb("p6_lg", [128, 8], F32)
            m8 = A.sb("p6_m8", [128, 8], F32)
            nm1 = A.sb("p6_nm1", [128, 1], F32)
            e2 = A.sb("p6_e2", [128, 1], F32)
            msk = A.sb("p6_msk", [128, 8], F32)
            PG = [A.ps(f"p6_PG{i}", [128, 512], F32) for i in range(2)]
            PU = [A.ps(f"p6_PU{i}", [128, 512], F32) for i in range(2)]
            PY = [A.ps(f"p6_PY{i}", [128, 512], F32) for i in range(3)]
            PR = A.ps("p6_PR", [128, 512], F32)
            em.dma("sp", l2w[:], row_bc(I["ln2_w"][layer], D), writes=["p6_l2w"])
            em.dma("act", l2b[:], row_bc(I["ln2_b"][layer], D), writes=["p6_l2b"])
            if moe:
                em.dma("sp", rws[:], I["router_w"].rearrange("(k p) e -> p k e", p=128), writes=["p6_rws"])
                em.op("dve", lambda: nc.vector.tensor_copy(out=rwb[:], in_=rws[:]), reads=["p6_rws"], writes=["p6_rwb"])
            wi = 0
            gi = 0
            yi = 0
            for tt in range(t0, n_tt):
                tcols = slice(tt * 1024, (tt + 1) * 1024)
                for k in range(8):
                    em.dma(("sp", "act")[k % 2], u2T[:, k, :], I["u2T_d"][k, :, tcols], reads=["u2T_d"], writes=["p6_u2T"])
                if moe:
                    for sub in range(8):
                        for k in range(8):
                            em.op("pe", lambda k=k, sub=sub: nc.tensor.matmul(PR[:, 0:8], lhsT=u2T[:, k, sub * 128:(sub + 1) * 128], rhs=rwb[:, k, :], start=(k == 0), stop=(k == 7)),
                                  reads=["p6_u2T", "p6_rwb"], writes=["p6_PR"])
                        em.op("act", lambda: nc.scalar.copy(out=lg[:], in_=PR[:, 0:8]), reads=["p6_PR"], writes=["p6_lg"])
                        em.op("dve", lambda: nc.vector.max(out=m8[:], in_=lg[:]), reads=["p6_lg"], writes=["p6_m8"])
                        em.op("dve", lambda: nc.vector.tensor_scalar(out=nm1[:], in0=m8[:, 0:1], scalar1=-1.0, scalar2=None, op0=ALU.mult), reads=["p6_m8"], writes=["p6_nm1"])
                        em.op("act", lambda: nc.scalar.activation(out=e2[:], in_=m8[:, 1:2], func=AF.Exp, bias=nm1[:, 0:1]), reads=["p6_m8", "p6_nm1"], writes=["p6_e2"])
                        em.op("dve", lambda: nc.vector.tensor_scalar(out=e2[:], in0=e2[:], scalar1=1.0, scalar2=None, op0=ALU.add), reads=["p6_e2"], writes=["p6_e2"])
                        em.op("dve", lambda: nc.vector.reciprocal(out=e2[:], in_=e2[:]), reads=["p6_e2"], writes=["p6_e2"])
                        em.op("dve", lambda: nc.vector.tensor_scalar(out=msk[:], in0=lg[:], scalar1=m8[:, 1:2], scalar2=e2[:, 0:1], op0=ALU.is_ge, op1=ALU.mult), reads=["p6_lg", "p6_m8", "p6_e2"], writes=["p6_msk"])
                        em.op("act", lambda: nc.scalar.activation(out=lg[:], in_=lg[:], func=AF.Exp, bias=nm1[:, 0:1]), reads=["p6_lg", "p6_nm1"], writes=["p6_lg"])
                        em.op("dve", lambda sub=sub: nc.vector.tensor_tensor(out=G[:, sub, :], in0=lg[:], in1=msk[:], op=ALU.mult), reads=["p6_lg", "p6_msk"], writes=["p6_G"])
                first = True
                for e in range(NE):
                    wgsrc = I["exp_w_gate"][e] if moe else I["ffn_w_gate"][0]
                    wusrc = I["exp_w_up"][e] if moe else I["ffn_w_up"][0]
                    wdsrc = I["exp_w_down"][e] if moe else I["ffn_w_down"][0]
                    for fp in range(0, NFG, 2):
                        fgs = [fg for fg in (fp, fp + 1) if fg < NFG]
                        for b, fg in enumerate(fgs):
                            f0 = fg * 256
                            em.dma("sp", wgs[b][:], wgsrc[:, f0:f0 + 256].rearrange("(k p) f -> p k f", p=128), writes=[f"p6_wgs{b}"])
                            em.dma("act", wus[b][:], wusrc[:, f0:f0 + 256].rearrange("(k p) f -> p k f", p=128), writes=[f"p6_wus{b}"])
                            em.dma("sp", wds[b][:], wdsrc[f0:f0 + 256, :].rearrange("(c p) d -> p c d", p=128), writes=[f"p6_wds{b}"])
                            em.op("act", lambda b=b: nc.scalar.copy(out=wgb[b][:], in_=wgs[b][:]), reads=[f"p6_wgs{b}"], writes=[f"p6_wgb{b}"])
                            em.op("act", lambda b=b: nc.scalar.copy(out=wub[b][:], in_=wus[b][:]), reads=[f"p6_wus{b}"], writes=[f"p6_wub{b}"])
                            em.op("dve", lambda b=b: nc.vector.tensor_copy(out=wdb[b][:, 0, :], in_=wds[b][:, 0, :]), reads=[f"p6_wds{b}"], writes=[f"p6_wdb{b}"])
                            em.op("pool", lambda b=b: nc.gpsimd.tensor_copy(out=wdb[b][:, 1, :], in_=wds[b][:, 1, :]), reads=[f"p6_wds{b}"], writes=[f"p6_wdb{b}"])
                            for fc in range(2):
                                for hf in range(2):
                                    pb_ = gi % 2
                                    gi += 1
                                    for k in range(8):
                                        em.op("pe", lambda k=k, fc=fc, hf=hf, b=b, pb_=pb_: nc.tensor.matmul(PG[pb_][:], lhsT=wgb[b][:, k, fc * 128:(fc + 1) * 128], rhs=u2T[:, k, hf * 512:(hf + 1) * 512], start=(k == 0), stop=(k == 7)),
                                              reads=[f"p6_wgb{b}", "p6_u2T"], writes=[f"p6_PG{pb_}"])
                                    for k in range(8):
                                        em.op("pe", lambda k=k, fc=fc, hf=hf, b=b, pb_=pb_: nc.tensor.matmul(PU[pb_][:], lhsT=wub[b][:, k, fc * 128:(fc + 1) * 128], rhs=u2T[:, k, hf * 512:(hf + 1) * 512], start=(k == 0), stop=(k == 7)),
                                              reads=[f"p6_wub{b}", "p6_u2T"], writes=[f"p6_PU{pb_}"])
                                    em.op("act", lambda pb_=pb_: nc.scalar.activation(out=sgs[pb_][:], in_=PG[pb_][:], func=AF.Silu), reads=[f"p6_PG{pb_}"], writes=[f"p6_sg{pb_}"])
                                    em.op("dve", lambda pb_=pb_, fc=fc, hf=hf, b=b: nc.vector.tensor_tensor(out=hTs[b][:, fc, hf * 512:(hf + 1) * 512], in0=sgs[pb_][:], in1=PU[pb_][:], op=ALU.mult),
                                          reads=[f"p6_sg{pb_}", f"p6_PU{pb_}"], writes=[f"p6_hT{b}"])
                        chunks = [(b, fc) for b in range(len(fgs)) for fc in range(2)]
                        for sub in range(8):
                            for h2 in range(2):
                                yb = yi % 3
                                yi += 1
                                for ci, (b, fc) in enumerate(chunks):
                                    em.op("pe", lambda fc=fc, sub=sub, h2=h2, b=b, yb=yb, ci=ci: nc.tensor.matmul(PY[yb][:], lhsT=hTs[b][:, fc, sub * 128:(sub + 1) * 128], rhs=wdb[b][:, fc, h2 * 512:(h2 + 1) * 512], start=(ci == 0), stop=(ci == len(chunks) - 1)),
                                          reads=[f"p6_hT{b}", f"p6_wdb{b}"], writes=[f"p6_PY{yb}"])
                                dsta = acc[:, sub, h2 * 512:(h2 + 1) * 512]
                                ak = f"p6_acc{sub}_{h2}"
                                if moe:
                                    if first:
                                        em.op("dve", lambda yb=yb, sub=sub, e=e, dsta=dsta: nc.vector.tensor_scalar(out=dsta, in0=PY[yb][:], scalar1=G[:, sub, e:e + 1], scalar2=None, op0=ALU.mult),
                                              reads=[f"p6_PY{yb}", "p6_G"], writes=[ak])
                                    else:
                                        em.op("dve", lambda yb=yb, sub=sub, e=e, dsta=dsta: nc.vector.scalar_tensor_tensor(out=dsta, in0=PY[yb][:], scalar=G[:, sub, e:e + 1], in1=dsta, op0=ALU.mult, op1=ALU.add),
                                              reads=[f"p6_PY{yb}", "p6_G", ak], writes=[ak])
                                else:
                                    if first:
                                        em.op("dve", lambda yb=yb, dsta=dsta: nc.vector.tensor_copy(out=dsta, in_=PY[yb][:]), reads=[f"p6_PY{yb}"], writes=[ak])
                                    else:
                                        em.op("dve", lambda yb=yb, dsta=dsta: nc.vector.tensor_tensor(out=dsta, in0=PY[yb][:], in1=dsta, op=ALU.add), reads=[f"p6_PY{yb}", ak], writes=[ak])
                        first = False
                for sub in range(8):
                    n = tt * 8 + sub
                    rows = slice(n * 128, (n + 1) * 128)
                    x1t, xk = x1s[n % 2], f"p6_x1{n % 2}"
                    em.dma("act", x1t[:], I["x1_d"][rows, :], reads=["x1_d"], writes=[xk])
                    em.op("dve", lambda sub=sub: nc.vector.tensor_tensor(out=t[:], in0=acc[:, sub, :], in1=mod[:, 5120:6144], op=ALU.mult), reads=[f"p6_acc{sub}_0", f"p6_acc{sub}_1", "mod"], writes=["p6_t"])
                    em.op("dve", lambda x1t=x1t: nc.vector.scalar_tensor_tensor(out=t[:], in0=x1t[:], scalar=DN_ALPHA, in1=t[:], op0=ALU.mult, op1=ALU.add), reads=[xk, "p6_t"], writes=["p6_t"])
                    self.ln_stats(t, "p6_t", st, mv, rs, "p6_")
                    em.op("act", lambda: nc.scalar.activation(out=xn[:], in_=t[:], func=AF.Identity, scale=rs[:, 0:1], bias=mv[:, 1:2]), reads=["p6_t", "p6_mv", "p6_rs"], writes=["p6_xn"])
                    em.op("dve", lambda: nc.vector.tensor_tensor(out=xn[:], in0=xn[:], in1=l2w[:], op=ALU.mult), reads=["p6_xn", "p6_l2w"], writes=["p6_xn"])
                    em.op("pool", lambda: nc.gpsimd.tensor_tensor(out=ot[:], in0=xn[:], in1=l2b[:], op=ALU.add), reads=["p6_xn", "p6_l2b"], writes=["p6_ot"])
                    drows = slice((n - t0 * 8) * 128, (n - t0 * 8 + 1) * 128)
                    em.dma("sp", dst[drows, :], ot[:], reads=["p6_ot"], writes=["dst%d" % layer], is_output=is_output)

    def build_all(self):
        nc = self.nc
        out = self.make_out()
        with nc.sbuf_tensor("mod", [128, 6 * D], F32) as mod:
            for layer in range(DEPTH):
                x_src = self.I["x"] if layer == 0 else self.I["x2_d"]
                self.phase0(layer, mod)
                self.phase1(layer, x_src, mod)
                self.phase2(layer)
                self.phase3(layer)
                self.phase4(layer)
                self.phase5(layer, x_src, mod)
                self.phase6(layer, mod, moe=(layer % 2 == 1), dst=(out if layer == DEPTH - 1 else self.I["x2_d"]), is_output=(layer == DEPTH - 1))
            self.em.barrier()
            self.em.finish()


def _bf(a):
    return np.asarray(a, dtype=np.float32).astype(ml_dtypes.bfloat16)


def make_consts():
    c = {}
    c["ident"] = _bf(np.eye(128))
    j = np.arange(128)[:, None]
    k = np.arange(S)[None, :]
    c["E_all"] = _bf((k // 64 == j).astype(np.float32))
    cc = np.arange(512)[:, None]
    jj = np.arange(128)[None, :]
    ov = ((cc * 16 < jj * 64 + 64) & (cc * 16 + 32 > jj * 64) & (cc < 511)).astype(np.float32)
    c["overlap"] = _bf(ov)
    cl = np.arange(128)[:, None]
    ql = np.arange(128)[None, :]
    c["cmaskb"] = _bf(np.stack([np.where(16 * cl + 31 <= 128 * pi + ql, 0.0, NEGB) for pi in range(17)]))
    c["cb"] = _bf(np.where(cl <= ql, 0.0, NEGB))
    c["wlo"] = _bf(np.where(cl > ql, 0.0, NEGB))
    bonus = np.zeros((NT, 128, 128), np.float32)
    qq = np.arange(128)[:, None]
    jb = np.arange(128)[None, :]
    for n in range(NT):
        i = 2 * n + (qq >= 64)
        forced = (jb == 0) | (jb == i) | (jb == i - 1)
        bonus[n] = np.where(jb <= i, np.where(forced, 1e4, 0.0), -1e30)
    c["bonus"] = bonus
    c["tri"] = (cl <= ql).astype(np.float32)
    c["tris"] = (cl > ql).astype(np.float32)
    c["ones"] = np.ones((128, 128), np.float32)
    c["tri_bf"] = _bf(c["tri"])
    c["ident_f"] = np.eye(128, dtype=np.float32)
    return c


def arrange_w_in(w_in):
    L = w_in.shape[0]
    o = {}
    off = 0
    for name, sz in (("q", 512), ("kc", 128), ("vc", 128), ("ks", 128), ("vs", 128), ("kw", 128), ("vw", 128), ("g", 24), ("z", 512), ("xbc", 1024), ("dt", 8)):
        o[name] = (off, off + sz)
        off += sz
    cols = []
    for r in range(4):
        for g in range(2):
            h = g * 4 + r
            cols += list(range(o["q"][0] + h * 64, o["q"][0] + (h + 1) * 64))
    cols += list(range(*o["ks"]))
    cols += list(range(*o["kw"]))
    for nm in ("kc", "vc"):
        for g in range(2):
            blk = list(range(o[nm][0] + g * 64, o[nm][0] + (g + 1) * 64))
            cols += blk + blk
    cols += list(range(*o["xbc"]))
    cols += list(range(*o["vs"])) + list(range(*o["vw"])) + list(range(*o["g"])) + list(range(*o["dt"]))
    cols += list(range(*o["z"]))
    assert len(cols) == W1C
    return np.ascontiguousarray(w_in[:, :, np.asarray(cols)])


def host_inputs(inputs):
    f = lambda a: np.ascontiguousarray(np.asarray(a, dtype=np.float32))
    sh = {}
    sh["ada_w"] = f(inputs["ada_w"]); sh["ada_b"] = f(inputs["ada_b"])
    sh["w_in_r"] = arrange_w_in(f(inputs["w_in"]))
    for kv in "kv":
        sh[f"cmp_pos_{kv}"] = np.ascontiguousarray(f(inputs[f"cmp_pos_{kv}"]).reshape(2, 16, 128).transpose(0, 2, 1))
        sh[f"cmp_w1_{kv}"] = f(inputs[f"cmp_w1_{kv}"]); sh[f"cmp_w2_{kv}"] = f(inputs[f"cmp_w2_{kv}"])
    sh["conv_w_r"] = np.ascontiguousarray(f(inputs["conv_w"]).reshape(2, 4, 8, 128).transpose(0, 3, 2, 1))
    sh["conv_b_r"] = np.ascontiguousarray(f(inputs["conv_b"]).reshape(2, 8, 128).transpose(0, 2, 1))
    for n in ("attn_norm_w", "dt_bias", "a_log", "d_skip", "ssm_norm_w", "w_out", "ln1_w", "ln1_b", "ln2_w", "ln2_b",
              "ffn_w_gate", "ffn_w_up", "ffn_w_down"):
        sh[n] = f(inputs[n])
    sh["router_w"] = f(inputs["router_w"])[0]
    sh["exp_w_gate"] = f(inputs["exp_w_gate"])[0]; sh["exp_w_up"] = f(inputs["exp_w_up"])[0]; sh["exp_w_down"] = f(inputs["exp_w_down"])[0]
    sh.update(make_consts())
    x = f(inputs["x"]); c = f(inputs["c"])
    per = []
    for b in range(x.shape[0]):
        per.append({"x": x[b], "cT": np.ascontiguousarray(c[b].reshape(8, 128).T)})
    return sh, per


_CACHE = {}

MODE = "multi"

_SET_A = ["qT_d", "kselT_d", "kwinT_d", "vsel_d", "vwin_d", "gates_d", "kcT_d", "vc_d", "ssm_d"]
SPLIT_N = 40


def _build_launch(idx):
    if idx in (0, 1):
        layer = idx
        ein = ["x2_d"] if layer == 1 else []
        eout = ["x2_d"] if layer == 0 else ["x1_d", "u2T_d"]
        kb = K(ext_in=ein, ext_out=eout)
        kb.declare()
        nc = kb.nc
        with nc.sbuf_tensor("mod", [128, 6 * D], F32) as mod:
            x_src = kb.I["x"] if layer == 0 else kb.I["x2_d"]
            kb.phase0(layer, mod)
            kb.phase1(layer, x_src, mod)
            kb.phase2(layer)
            kb.phase4(layer)
            kb.phase3(layer)
            kb.phase5(layer, x_src, mod)
            if layer == 0:
                kb.phase6(layer, mod, moe=False, dst=kb.I["x2_d"])
            kb.em.barrier(); kb.em.finish()
        return kb, ein, eout
    t0, t1 = {2: (0, 4), 3: (4, 8)}[idx]
    ein = ["x1_d", "u2T_d"]
    oname = "out_%d" % idx
    eout = [oname]
    kb = K(ext_in=ein, ext_out=eout)
    kb.declare()
    kb.stab[oname] = ([(t1 - t0) * 1024, D], F32)
    nc = kb.nc
    with nc.sbuf_tensor("mod", [128, 6 * D], F32) as mod:
        kb.phase0(1, mod)
        kb.phase6(1, mod, moe=True, dst=kb.I[oname], n_tt=t1, t0=t0, is_output=True)
        kb.em.barrier(); kb.em.finish()
    return kb, ein, eout


def _run(kb, sh, per, hand, n_cores):
    names = [n for n in kb.I.keys() if (n in kb.itab or n in kb.ext_in)]
    in_maps = []
    for b in range(n_cores):
        m = {}
        for n in names:
            if n in kb.ext_in:
                m[n] = hand[b][n]
            else:
                m[n] = per[b][n] if n in per[b] else sh[n]
        in_maps.append(m)
    return run_bass_kernel_spmd(kb.nc, in_maps, core_ids=list(range(n_cores)))


def kernel(**inputs):
    sh, per = host_inputs(inputs)
    nb = len(per)
    if MODE == "fused":
        if "kb" not in _CACHE:
            kb = K()
            kb.declare()
            kb.build_all()
            _CACHE["kb"] = kb
        kb = _CACHE["kb"]
        res = _run(kb, sh, per, None, nb)
        return np.stack([np.asarray(r["out"], dtype=np.float32) for r in res.results], axis=0)
    hand = [dict() for _ in range(nb)]
    for idx in range(4):
        key = "L%d" % idx
        if key not in _CACHE:
            _CACHE[key] = _build_launch(idx)
        kb, ein, eout = _CACHE[key]
        res = _run(kb, sh, per, hand, nb)
        for b in range(nb):
            for n in eout:
                hand[b][n] = res.results[b][n]
    outs = []
    for b in range(nb):
        outs.append(np.concatenate([np.asarray(hand[b]["out_%d" % i], dtype=np.float32) for i in (2, 3)], axis=0))
    return np.stack(outs, axis=0)
```

```python
import numpy as np
import ml_dtypes
import concourse.bass as bass
import concourse.mybir as mybir
from concourse.bass_types import AP
from concourse.bass_utils import run_bass_kernel_spmd

F32 = mybir.dt.float32
BF16 = mybir.dt.bfloat16
AF = mybir.ActivationFunctionType
ALU = mybir.AluOpType

S = 8192
D = 1024
NT = S // 128
DEPTH = 2
DN_ALPHA = (2 * DEPTH) ** 0.25
LN_EPS = 1e-5
RMS_EPS = 1e-6
NEGB = -30000.0
W1C = 18 * 128 + 288 + 512
FFN_DENSE = 2816
FFN_EXPERT = 3584
NEXP = 8
EPOCH = 12000


import re
_PSUM_KEY = re.compile(r"^p\d_(ps\d|pT|pa|pb|pf\d|pp|ph|po|S\d|OC|IMP|OS|OW|TP2?|PA|PSc|PD\d|PY\d?|PI|PSt|H\d|PG\d|PU\d|PR)$")


class Em:
    def __init__(self, nc, n_dma_sems=40):
        self.nc = nc
        self.eng = {"pe": nc.tensor, "act": nc.scalar, "dve": nc.vector, "pool": nc.gpsimd, "sp": nc.sync}
        self.cur = {}
        self.nsem = 0
        for e in ("pe", "act", "dve", "pool"):
            self.cur[e] = [self._new_sem(e), 0]
        self.dma_sems = [[self._new_sem("dma"), 0] for _ in range(n_dma_sems)]
        self.dma_rr = 0
        self.seen = {e: {} for e in self.eng}
        self.last_w = {}
        self.readers = {}
        self.ninstr = 0
        self.out_events = []

    def _new_sem(self, tag):
        self.nsem += 1
        return self.nc.alloc_semaphore(f"s_{tag}_{self.nsem}")

    def _wait(self, engname, ev):
        sem, val, src = ev
        sid = id(sem)
        if self.seen[engname].get(sid, 0) >= val:
            return
        self.eng[engname].wait_ge(sem, val)
        self.seen[engname][sid] = val

    def _record(self, ev, reads, writes):
        for k in writes:
            if ev[2] == "dma":
                d = {}
                for e in list(self.last_w.get(k, ())) + [ev]:
                    if e[2] == "dma" and (id(e[0]) not in d or d[id(e[0])][1] < e[1]):
                        d[id(e[0])] = e
                self.last_w[k] = list(d.values())
            else:
                self.last_w[k] = [ev]
            self.readers[k] = []
        for k in reads:
            lst = self.readers.setdefault(k, [])
            lst.append(ev)
            if len(lst) > 8:
                d = {}
                for e in lst:
                    kk = id(e[0])
                    if kk not in d or d[kk][1] < e[1]:
                        d[kk] = e
                self.readers[k] = list(d.values())

    def op(self, engname, fn, reads=(), writes=()):
        for k in reads:
            for ev in self.last_w.get(k, ()):
                self._wait(engname, ev)
            if _PSUM_KEY.match(k):
                for rv in self.readers.get(k, ()):
                    if rv[2] != engname:
                        self._wait(engname, rv)
        for k in writes:
            for ev in self.last_w.get(k, ()):
                if ev[2] != engname or engname != "pe":
                    self._wait(engname, ev)
            for rv in self.readers.get(k, ()):
                if rv[2] != engname:
                    self._wait(engname, rv)
        c = self.cur[engname]
        if c[1] >= EPOCH:
            c[0] = self._new_sem(engname)
            c[1] = 0
        ins = fn()
        ins.then_inc(c[0], 1)
        c[1] += 1
        ev = (c[0], c[1], engname)
        self._record(ev, reads, writes)
        self.ninstr += 1
        return ev

    def dma(self, queue, out, in_, reads=(), writes=(), is_output=False):
        slot = self.dma_sems[self.dma_rr]
        self.dma_rr = (self.dma_rr + 1) % len(self.dma_sems)
        if slot[1] > 0:
            self._wait(queue, (slot[0], slot[1], "dma"))
        for k in reads:
            for ev in self.last_w.get(k, ()):
                self._wait(queue, ev)
        for k in writes:
            for ev in self.last_w.get(k, ()):
                if ev[2] != "dma":
                    self._wait(queue, ev)
            for rv in self.readers.get(k, ()):
                self._wait(queue, rv)
        ins = self.eng[queue].dma_start(out=out, in_=in_)
        slot[1] += 16
        ins.then_inc(slot[0], 16)
        ev = (slot[0], slot[1], "dma")
        self._record(ev, reads, writes)
        self.ninstr += 1
        if is_output:
            self.out_events.append(ev)
        return ev

    def barrier(self):
        for w in ("sp", "act", "pool", "pe", "dve"):
            for slot in self.dma_sems:
                if slot[1] > 0:
                    self._wait(w, (slot[0], slot[1], "dma"))
            for e in ("pe", "act", "dve", "pool"):
                c = self.cur[e]
                if c[1] > 0 and e != w:
                    self._wait(w, (c[0], c[1], e))

    def finish(self):
        for slot in self.dma_sems:
            if slot[1] > 0:
                self._wait("sp", (slot[0], slot[1], "dma"))
        for e in ("pe", "act", "dve", "pool"):
            c = self.cur[e]
            if c[1] > 0:
                self._wait("sp", (c[0], c[1], e))


class Alloc:
    def __init__(self, nc, em=None):
        import contextlib
        self.nc = nc
        self.em = em
        self.st = contextlib.ExitStack()

    def __enter__(self):
        self.st.__enter__()
        return self

    def __exit__(self, *a):
        if self.em is not None and a[0] is None:
            self.em.barrier()
        return self.st.__exit__(*a)

    _uid = [0]

    def sb(self, name, shape, dt):
        Alloc._uid[0] += 1
        return self.st.enter_context(self.nc.sbuf_tensor(f"{name}_{Alloc._uid[0]}", list(shape), dt))

    def ps(self, name, shape, dt):
        Alloc._uid[0] += 1
        return self.st.enter_context(self.nc.psum_tensor(f"{name}_{Alloc._uid[0]}", list(shape), dt))


def bc(ap_, shape, axis):
    return ap_.unsqueeze(axis).to_broadcast(shape)


def row_bc(dram_ap, n):
    return AP(dram_ap.tensor, dram_ap.offset, [[0, 128], [1, n]])


class K:
    def __init__(self, dbg=(), ext_in=(), ext_out=()):
        self.nc = nc = bass.Bass("TRN2", target_bir_lowering=False)
        self.em = Em(nc)
        self.dbg = set(dbg) | set(ext_out)
        self.ext_in = set(ext_in)
        self.I = {}
        self.Sx = {}
        self.qrr = 0

    def inp(self, name, shape, dt=F32):
        t = self.nc.dram_tensor(name, list(shape), dt, kind="ExternalInput").ap()
        return t

    def scratch(self, name, shape, dt):
        kind = "ExternalOutput" if name in self.dbg else ("ExternalInput" if name in self.ext_in else "Internal")
        t = self.nc.dram_tensor(name, list(shape), dt, kind=kind).ap()
        return t

    def q(self):
        self.qrr += 1
        return ("sp", "act")[self.qrr % 2]

    def declare(self):
        T = {}
        def I(name, shape, dt=F32):
            T[name] = (shape, dt)
        I("x", [S, D]); I("cT", [128, 8])
        I("ada_w", [2, D, 6 * D]); I("ada_b", [2, 6 * D])
        I("w_in_r", [2, D, W1C])
        for kv in "kv":
            I(f"cmp_pos_{kv}", [2, 128, 16]); I(f"cmp_w1_{kv}", [2, 2048, 256]); I(f"cmp_w2_{kv}", [2, 256, 64])
        I("attn_norm_w", [2, 512]); I("conv_w_r", [2, 128, 8, 4]); I("conv_b_r", [2, 128, 8])
        I("dt_bias", [2, 8]); I("a_log", [2, 8]); I("d_skip", [2, 8]); I("ssm_norm_w", [2, 512])
        I("w_out", [2, D, D])
        for n in ("ln1_w", "ln1_b", "ln2_w", "ln2_b"):
            I(n, [2, D])
        I("ffn_w_gate", [1, D, FFN_DENSE]); I("ffn_w_up", [1, D, FFN_DENSE]); I("ffn_w_down", [1, FFN_DENSE, D])
        I("router_w", [D, NEXP])
        I("exp_w_gate", [NEXP, D, FFN_EXPERT]); I("exp_w_up", [NEXP, D, FFN_EXPERT]); I("exp_w_down", [NEXP, FFN_EXPERT, D])
        I("ident", [128, 128], BF16); I("E_all", [128, S], BF16); I("overlap", [512, 128], BF16)
        I("cmaskb", [17, 128, 128], BF16); I("cb", [128, 128], BF16); I("wlo", [128, 128], BF16)
        I("bonus", [NT, 128, 128]); I("tri", [128, 128]); I("tris", [128, 128]); I("ones", [128, 128])
        I("tri_bf", [128, 128], BF16)
        self.itab = T
        ST = {}
        def Sc(name, shape, dt):
            ST[name] = (shape, dt)
        Sc("qT_d", [4, 128, S], BF16); Sc("kselT_d", [128, S], BF16); Sc("kwinT_d", [128, S], BF16)
        Sc("kcr_d", [2, 128, S], BF16); Sc("vcr_d", [2, 128, S], BF16)
        Sc("xbcT_d", [8, 128, S], BF16); Sc("xbc2T_d", [8, 128, S], BF16)
        Sc("vsel_d", [S, 128], BF16); Sc("vwin_d", [S, 128], BF16)
        Sc("gates_d", [S, 24], F32); Sc("z_d", [S, 512], BF16); Sc("dt_d", [S, 8], F32)
        Sc("kcT_d", [128, 512], BF16); Sc("vc_d", [512, 128], BF16)
        Sc("cat_d", [S, D], BF16); Sc("ssm_d", [S, 512], BF16); Sc("att_d", [S, 512], BF16)
        Sc("x1_d", [S, D], F32); Sc("x2_d", [S, D], F32)
        Sc("u2T_d", [8, 128, S], BF16)
        self.stab = ST
        k = self

        class _L(dict):
            def __missing__(d, name):
                if name in T:
                    v = k.inp(name, *T[name])
                else:
                    v = k.scratch(name, *ST[name])
                d[name] = v
                return v
        self.I = _L()
        self.Sx = self.I

    def make_out(self):
        self.out = self.nc.dram_tensor("out", [S, D], F32, kind="ExternalOutput").ap()
        return self.out

    def phase0(self, layer, mod):
        nc, em, I = self.nc, self.em, self.I
        with Alloc(nc, em) as A:
            ct = A.sb("p0_c", [128, 8], F32)
            sg = A.sb("p0_sig", [128, 8], F32)
            cbc = A.sb("p0_cbc", [128, 8, 128], F32)
            w0 = A.sb("p0_w0", [128, 8, 512], F32)
            w1 = A.sb("p0_w1", [128, 8, 512], F32)
            bt = A.sb("p0_b", [128, 6 * D], F32)
            ps0 = A.ps("p0_ps0", [128, 512], F32)
            ps1 = A.ps("p0_ps1", [128, 512], F32)
            em.dma("sp", ct[:], I["cT"], writes=["p0_c"])
            em.dma("act", bt[:], row_bc(I["ada_b"][layer], 6 * D), writes=["p0_b"])
            em.op("act", lambda: nc.scalar.activation(out=sg[:], in_=ct[:], func=AF.Exp, scale=-1.0), reads=["p0_c"], writes=["p0_sig"])
            em.op("dve", lambda: nc.vector.tensor_scalar(out=sg[:], in0=sg[:], scalar1=1.0, scalar2=None, op0=ALU.add), reads=["p0_sig"], writes=["p0_sig"])
            em.op("dve", lambda: nc.vector.reciprocal(out=sg[:], in_=sg[:]), reads=["p0_sig"], writes=["p0_sig"])
            em.op("dve", lambda: nc.vector.tensor_tensor(out=ct[:], in0=ct[:], in1=sg[:], op=ALU.mult), reads=["p0_sig", "p0_c"], writes=["p0_c"])
            em.op("dve", lambda: nc.vector.tensor_copy(out=cbc[:], in_=bc(ct[:], [128, 8, 128], 2)), reads=["p0_c"], writes=["p0_cbc"])
            wb = [(w0, "p0_w0"), (w1, "p0_w1")]
            pss = [(ps0, "p0_ps0"), (ps1, "p0_ps1")]
            for j in range(12):
                wt, wk = wb[j % 2]
                ps, pk = pss[j % 2]
                src = I["ada_w"][layer, :, j * 512:(j + 1) * 512].rearrange("(k p) n -> p k n", p=128)
                em.dma(("sp", "act")[j % 2], wt[:], src, writes=[wk])
                for k in range(8):
                    em.op("pe", lambda k=k, wt=wt, ps=ps: nc.tensor.matmul(ps[:], lhsT=cbc[:, k, :], rhs=wt[:, k, :], start=(k == 0), stop=(k == 7)),
                          reads=[wk, "p0_cbc"], writes=[pk])
                seg = j // 2
                em.op("dve", lambda j=j, ps=ps: nc.vector.tensor_tensor(out=mod[:, j * 512:(j + 1) * 512], in0=ps[:], in1=bt[:, j * 512:(j + 1) * 512], op=ALU.add),
                      reads=[pk, "p0_b"], writes=["mod"])
                if seg in (1, 2, 4, 5):
                    em.op("dve", lambda j=j: nc.vector.tensor_scalar(out=mod[:, j * 512:(j + 1) * 512], in0=mod[:, j * 512:(j + 1) * 512], scalar1=1.0, scalar2=None, op0=ALU.add),
                          reads=["mod"], writes=["mod"])

    def ln_stats(self, xt, xk, st, mv, rstd, key, eps=LN_EPS):
        nc, em = self.nc, self.em
        for h in range(2):
            em.op("dve", lambda h=h: nc.vector.bn_stats(out=st[:, h, :], in_=xt[:, h * 512:(h + 1) * 512]), reads=[xk], writes=[key + "st"])
        em.op("dve", lambda: nc.vector.bn_aggr(out=mv[:], in_=st[:].rearrange("p a b -> p (a b)")), reads=[key + "st"], writes=[key + "mv"])
        em.op("act", lambda: nc.scalar.activation(out=rstd[:], in_=mv[:, 1:2], func=AF.Ln, bias=eps), reads=[key + "mv"], writes=[key + "rs"])
        em.op("act", lambda: nc.scalar.activation(out=rstd[:], in_=rstd[:], func=AF.Exp, scale=-0.5), reads=[key + "rs"], writes=[key + "rs"])
        em.op("dve", lambda: nc.vector.tensor_scalar(out=mv[:, 1:2], in0=mv[:, 0:1], scalar1=rstd[:, 0:1], scalar2=-1.0, op0=ALU.mult, op1=ALU.mult), reads=[key + "mv", key + "rs"], writes=[key + "mv"])

    def phase1(self, layer, x_src, mod, n_super=16):
        nc, em, I, Sx = self.nc, self.em, self.I, self.Sx
        with Alloc(nc, em) as A:
            w = A.sb("p1_w", [128, 8, W1C], BF16)
            ws0 = A.sb("p1_ws0", [128, W1C], F32)
            ws1 = A.sb("p1_ws1", [128, W1C], F32)
            idt = A.sb("p1_id", [128, 128], BF16)
            dtb = A.sb("p1_dtb", [128, 8], F32)
            x0 = A.sb("p1_x0", [128, D], F32)
            x1 = A.sb("p1_x1", [128, D], F32)
            st = A.sb("p1_st", [128, 2, 6], F32)
            mv = A.sb("p1_mv", [128, 2], F32)
            rs = A.sb("p1_rs", [128, 1], F32)
            xn = A.sb("p1_xn", [128, D], F32)
            ub = A.sb("p1_ub", [128, D], BF16)
            uT0 = A.sb("p1_uT0", [128, 8, 512], BF16)
            uT1 = A.sb("p1_uT1", [128, 8, 512], BF16)
            sv = A.sb("p1_sv", [128, 4, 256], BF16)
            sgt = A.sb("p1_sg", [128, 4, 24], F32)
            sdt = A.sb("p1_sd", [128, 4, 8], F32)
            sz = A.sb("p1_sz", [128, 4, 512], BF16)
            f0 = A.sb("p1_f0", [128, 512], BF16)
            f1 = A.sb("p1_f1", [128, 512], BF16)
            f2 = A.sb("p1_f2", [128, 512], BF16)
            f3 = A.sb("p1_f3", [128, 512], BF16)
            pT = A.ps("p1_pT", [128, 1024], BF16)
            pa = A.ps("p1_pa", [128, 512], F32)
            pb = A.ps("p1_pb", [128, 512], F32)
            pf0 = A.ps("p1_pf0", [128, 512], F32)
            pf1 = A.ps("p1_pf1", [128, 512], F32)
            pf2 = A.ps("p1_pf2", [128, 512], F32)
            em.dma("sp", idt[:], I["ident"], writes=["p1_id"])
            em.dma("act", dtb[:], row_bc(I["dt_bias"][layer], 8), writes=["p1_dtb"])
            wss = [(ws0, "p1_ws0"), (ws1, "p1_ws1")]
            for k in range(8):
                wst, wsk = wss[k % 2]
                em.dma(("sp", "act")[k % 2], wst[:], I["w_in_r"][layer, k * 128:(k + 1) * 128, :], writes=[wsk])
                em.op("pool", lambda k=k, wst=wst: nc.gpsimd.tensor_copy(out=w[:, k, :], in_=wst[:]), reads=[wsk], writes=["p1_w"])
            STOP = 99
            xs = [(x0, "p1_x0"), (x1, "p1_x1")]
            uTs = [(uT0, "p1_uT0"), (uT1, "p1_uT1")]
            fst = [(f0, "p1_f0"), (f1, "p1_f1"), (f2, "p1_f2"), (f3, "p1_f3")]
            pfs = [(pf0, "p1_pf0"), (pf1, "p1_pf1"), (pf2, "p1_pf2")]
            fi = 0
            for T in range(n_super):
                uT, uk = uTs[T % 2]
                for sub in range(4):
                    n = T * 4 + sub
                    xt, xk = xs[n % 2]
                    em.dma(("sp", "act")[n % 2], xt[:], x_src[n * 128:(n + 1) * 128, :], writes=[xk])
                    self.ln_stats(xt, xk, st, mv, rs, "p1_")
                    em.op("act", lambda xt=xt: nc.scalar.activation(out=xn[:], in_=xt[:], func=AF.Identity, scale=rs[:, 0:1], bias=mv[:, 1:2]),
                          reads=[xk, "p1_mv", "p1_rs"], writes=["p1_xn"])
                    em.op("dve", lambda: nc.vector.tensor_tensor(out=xn[:], in0=xn[:], in1=mod[:, 1024:2048], op=ALU.mult), reads=["p1_xn", "mod"], writes=["p1_xn"])
                    em.op("pool", lambda: nc.gpsimd.tensor_tensor(out=ub[:], in0=xn[:], in1=mod[:, 0:1024], op=ALU.add), reads=["p1_xn", "mod"], writes=["p1_ub"])
                    if STOP <= 1:
                        continue
                    for k in range(8):
                        em.op("pe", lambda k=k: nc.tensor.transpose(out=pT[:, k * 128:(k + 1) * 128], in_=ub[:, k * 128:(k + 1) * 128], identity=idt[:]),
                              reads=["p1_ub", "p1_id"], writes=["p1_pT"])
                    em.op("act", lambda uT=uT, sub=sub: nc.scalar.copy(out=uT[:, :, sub * 128:(sub + 1) * 128], in_=pT[:].rearrange("p (k t) -> p k t", t=128)),
                          reads=["p1_pT"], writes=[uk])
                    if STOP <= 2:
                        continue
                    for k in range(8):
                        em.op("pe", lambda k=k, uT=uT, sub=sub: nc.tensor.matmul(pa[:, 0:288], lhsT=uT[:, k, sub * 128:(sub + 1) * 128], rhs=w[:, k, 2304:2592], start=(k == 0), stop=(k == 7)),
                              reads=[uk, "p1_w"], writes=["p1_pa"])
                    for k in range(8):
                        em.op("pe", lambda k=k, uT=uT, sub=sub: nc.tensor.matmul(pb[:], lhsT=uT[:, k, sub * 128:(sub + 1) * 128], rhs=w[:, k, 2592:3104], start=(k == 0), stop=(k == 7)),
                              reads=[uk, "p1_w"], writes=["p1_pb"])
                    if STOP <= 3:
                        continue
                    em.op("dve", lambda sub=sub: nc.vector.tensor_copy(out=sv[:, sub, :], in_=pa[:, 0:256]), reads=["p1_pa"], writes=["p1_sv"])
                    em.op("act", lambda sub=sub: nc.scalar.activation(out=sgt[:, sub, :], in_=pa[:, 256:280], func=AF.Exp, scale=-1.0), reads=["p1_pa"], writes=["p1_sg"])
                    em.op("dve", lambda sub=sub: nc.vector.tensor_scalar(out=sgt[:, sub, :], in0=sgt[:, sub, :], scalar1=1.0, scalar2=None, op0=ALU.add), reads=["p1_sg"], writes=["p1_sg"])
                    em.op("dve", lambda sub=sub: nc.vector.reciprocal(out=sgt[:, sub, :], in_=sgt[:, sub, :]), reads=["p1_sg"], writes=["p1_sg"])
                    em.op("dve", lambda sub=sub: nc.vector.tensor_tensor(out=sdt[:, sub, :], in0=pa[:, 280:288], in1=dtb[:], op=ALU.add), reads=["p1_pa", "p1_dtb"], writes=["p1_sd"])
                    em.op("act", lambda sub=sub: nc.scalar.activation(out=sdt[:, sub, :], in_=sdt[:, sub, :], func=AF.Exp), reads=["p1_sd"], writes=["p1_sd"])
                    em.op("act", lambda sub=sub: nc.scalar.activation(out=sdt[:, sub, :], in_=sdt[:, sub, :], func=AF.Ln, bias=1.0), reads=["p1_sd"], writes=["p1_sd"])
                    em.op("act", lambda sub=sub: nc.scalar.copy(out=sz[:, sub, :], in_=pb[:]), reads=["p1_pb"], writes=["p1_sz"])
                if STOP <= 4:
                    continue
                rows = slice(T * 512, (T + 1) * 512)
                em.dma("sp", Sx["vsel_d"][rows, :].rearrange("(s p) c -> p s c", p=128), sv[:, :, 0:128], reads=["p1_sv"], writes=["vsel_d"])
                em.dma("act", Sx["vwin_d"][rows, :].rearrange("(s p) c -> p s c", p=128), sv[:, :, 128:256], reads=["p1_sv"], writes=["vwin_d"])
                em.dma("sp", Sx["gates_d"][rows, :].rearrange("(s p) c -> p s c", p=128), sgt[:], reads=["p1_sg"], writes=["gates_d"])
                em.dma("act", Sx["dt_d"][rows, :].rearrange("(s p) c -> p s c", p=128), sdt[:], reads=["p1_sd"], writes=["dt_d"])
                em.dma("sp", Sx["z_d"][rows, :].rearrange("(s p) c -> p s c", p=128), sz[:], reads=["p1_sz"], writes=["z_d"])
                if STOP <= 5:
                    continue
                cols = slice(T * 512, (T + 1) * 512)
                for ch in range(18):
                    pf, pfk = pfs[ch % 3]
                    fs, fk = fst[fi % 4]
                    fi += 1
                    for k in range(8):
                        em.op("pe", lambda k=k, ch=ch, pf=pf, uT=uT: nc.tensor.matmul(pf[:], lhsT=w[:, k, ch * 128:(ch + 1) * 128], rhs=uT[:, k, :], start=(k == 0), stop=(k == 7)),
                              reads=[uk, "p1_w"], writes=[pfk])
                    if ch < 4:
                        em.op("act", lambda pf=pf, fs=fs: nc.scalar.activation(out=fs[:], in_=pf[:], func=AF.Copy, scale=0.125), reads=[pfk], writes=[fk])
                    elif ch % 2 == 0:
                        em.op("dve", lambda pf=pf, fs=fs: nc.vector.tensor_copy(out=fs[:], in_=pf[:]), reads=[pfk], writes=[fk])
                    else:
                        em.op("act", lambda pf=pf, fs=fs: nc.scalar.copy(out=fs[:], in_=pf[:]), reads=[pfk], writes=[fk])
                    qn = ("sp", "act")[ch % 2]
                    if ch < 4:
                        em.dma(qn, Sx["qT_d"][ch, :, cols], fs[:], reads=[fk], writes=["qT_d"])
                    elif ch == 4:
                        em.dma(qn, Sx["kselT_d"][:, cols], fs[:], reads=[fk], writes=["kselT_d"])
                    elif ch == 5:
                        em.dma(qn, Sx["kwinT_d"][:, cols], fs[:], reads=[fk], writes=["kwinT_d"])
                    elif ch < 10:
                        dst = Sx["kcr_d"] if ch < 8 else Sx["vcr_d"]
                        dk = "kcr_d" if ch < 8 else "vcr_d"
                        g = ch % 2
                        em.dma(qn, dst[g, 0:64, cols], fs[0:64, :], reads=[fk], writes=[dk])
                        if T == 0:
                            em.dma(qn, dst[g, 64:128, 0:511], fs[64:128, 1:512], reads=[fk], writes=[dk])
                        else:
                            em.dma(qn, dst[g, 64:128, T * 512 - 1:T * 512 + 511], fs[64:128, :], reads=[fk], writes=[dk])
                    else:
                        em.dma(qn, Sx["xbcT_d"][ch - 10, :, cols], fs[:], reads=[fk], writes=["xbcT_d"])


    def phase2(self, layer):
        nc, em, I = self.nc, self.em, self.I
        with Alloc(nc, em) as A:
            w1s = A.sb("p2_w1s", [128, 16, 256], F32)
            w1b = A.sb("p2_w1b", [128, 16, 256], BF16)
            poss = A.sb("p2_poss", [128, 16], F32)
            posb = A.sb("p2_posb", [128, 16], BF16)
            w2s = A.sb("p2_w2s", [128, 2, 64], F32)
            w2b = A.sb("p2_w2b", [128, 2, 64], BF16)
            pbias = A.sb("p2_pb", [128, 2], F32)
            kv2 = A.sb("p2_kv2", [128, S], BF16)
            xg = A.sb("p2_xg", [128, 512], F32)
            tg = A.sb("p2_tg", [128, 512], F32)
            g1T = A.sb("p2_g1T", [128, 2, 512], BF16)
            kst = A.sb("p2_kst", [64, 512], BF16)
            vst = A.sb("p2_vst", [128, 4, 64], BF16)
            pp = A.ps("p2_pp", [128, 512], F32)
            ph = A.ps("p2_ph", [128, 512], F32)
            po = A.ps("p2_po", [128, 512], F32)
            em.op("dve", lambda: nc.vector.memset(g1T[:], 0.0), writes=["p2_g1T"])
            em.op("dve", lambda: nc.vector.memset(kst[:], 0.0), writes=["p2_kst"])
            for kv in "kv":
                em.dma("sp", w1s[:], I[f"cmp_w1_{kv}"][layer].rearrange("(m p) h -> p m h", p=128), writes=["p2_w1s"])
                em.dma("act", poss[:], I[f"cmp_pos_{kv}"][layer], writes=["p2_poss"])
                em.dma("act", w2s[:], I[f"cmp_w2_{kv}"][layer].rearrange("(c p) d -> p c d", p=128), writes=["p2_w2s"])
                em.op("pool", lambda: nc.gpsimd.tensor_copy(out=w1b[:], in_=w1s[:]), reads=["p2_w1s"], writes=["p2_w1b"])
                em.op("dve", lambda: nc.vector.tensor_copy(out=posb[:], in_=poss[:]), reads=["p2_poss"], writes=["p2_posb"])
                em.op("dve", lambda: nc.vector.tensor_copy(out=w2b[:], in_=w2s[:]), reads=["p2_w2s"], writes=["p2_w2b"])
                for hc in range(2):
                    for m in range(16):
                        em.op("pe", lambda hc=hc, m=m: nc.tensor.matmul(pp[:, hc:hc + 1], lhsT=w1b[:, m, hc * 128:(hc + 1) * 128], rhs=posb[:, m:m + 1], start=(m == 0), stop=(m == 15)),
                              reads=["p2_w1b", "p2_posb"], writes=["p2_pp"])
                em.op("dve", lambda: nc.vector.tensor_copy(out=pbias[:], in_=pp[:, 0:2]), reads=["p2_pp"], writes=["p2_pb"])
                src = I["kcr_d"] if kv == "k" else I["vcr_d"]
                sk = "kcr_d" if kv == "k" else "vcr_d"
                for g in range(2):
                    for j in range(4):
                        em.dma(("sp", "act")[j % 2], kv2[:, j * 2048:(j + 1) * 2048], src[g, :, j * 2048:(j + 1) * 2048], reads=[sk], writes=["p2_kv2"])
                    base = kv2[:]
                    for hc in range(2):
                        for m in range(16):
                            rhs = AP(base.tensor, base.offset + 2 * m, [[base.ap[0][0], 128], [16, 511]])
                            em.op("pe", lambda hc=hc, m=m, rhs=rhs: nc.tensor.matmul(ph[:, 0:511], lhsT=w1b[:, m, hc * 128:(hc + 1) * 128], rhs=rhs, start=(m == 0), stop=(m == 15)),
                                  reads=["p2_w1b", "p2_kv2"], writes=["p2_ph"])
                        em.op("act", lambda hc=hc: nc.scalar.activation(out=xg[:, 0:511], in_=ph[:, 0:511], func=AF.Identity, bias=pbias[:, hc:hc + 1]),
                              reads=["p2_ph", "p2_pb"], writes=["p2_xg"])
                        em.op("dve", lambda: nc.vector.tensor_tensor(out=tg[:, 0:511], in0=xg[:, 0:511], in1=xg[:, 0:511], op=ALU.mult), reads=["p2_xg"], writes=["p2_tg"])
                        em.op("dve", lambda: nc.vector.tensor_scalar(out=tg[:, 0:511], in0=tg[:, 0:511], scalar1=0.044715, scalar2=1.0, op0=ALU.mult, op1=ALU.add), reads=["p2_tg"], writes=["p2_tg"])
                        em.op("dve", lambda: nc.vector.tensor_tensor(out=tg[:, 0:511], in0=tg[:, 0:511], in1=xg[:, 0:511], op=ALU.mult), reads=["p2_tg", "p2_xg"], writes=["p2_tg"])
                        em.op("act", lambda: nc.scalar.activation(out=tg[:, 0:511], in_=tg[:, 0:511], func=AF.Exp, scale=-1.5957691216057308), reads=["p2_tg"], writes=["p2_tg"])
                        em.op("dve", lambda: nc.vector.tensor_scalar(out=tg[:, 0:511], in0=tg[:, 0:511], scalar1=1.0, scalar2=None, op0=ALU.add), reads=["p2_tg"], writes=["p2_tg"])
                        em.op("dve", lambda: nc.vector.reciprocal(out=tg[:, 0:511], in_=tg[:, 0:511]), reads=["p2_tg"], writes=["p2_tg"])
                        em.op("dve", lambda hc=hc: nc.vector.tensor_tensor(out=g1T[:, hc, 0:511], in0=tg[:, 0:511], in1=xg[:, 0:511], op=ALU.mult), reads=["p2_tg", "p2_xg"], writes=["p2_g1T"])
                    if kv == "k":
                        for hc in range(2):
                            em.op("pe", lambda hc=hc: nc.tensor.matmul(po[0:64, 0:511], lhsT=w2b[:, hc, :], rhs=g1T[:, hc, 0:511], start=(hc == 0), stop=(hc == 1)),
                                  reads=["p2_w2b", "p2_g1T"], writes=["p2_po"])
                        em.op("act", lambda: nc.scalar.copy(out=kst[:, 0:511], in_=po[0:64, 0:511]), reads=["p2_po"], writes=["p2_kst"])
                        em.dma("sp", I["kcT_d"][g * 64:(g + 1) * 64, :], kst[:], reads=["p2_kst"], writes=["kcT_d"])
                    else:
                        for ct in range(4):
                            for hc in range(2):
                                em.op("pe", lambda hc=hc, ct=ct: nc.tensor.matmul(po[:, ct * 64:(ct + 1) * 64], lhsT=g1T[:, hc, ct * 128:(ct + 1) * 128], rhs=w2b[:, hc, :], start=(hc == 0), stop=(hc == 1)),
                                      reads=["p2_w2b", "p2_g1T"], writes=["p2_po"])
                        em.op("act", lambda: nc.scalar.copy(out=vst[:], in_=po[:, 0:256].rearrange("p (c d) -> p c d", d=64)), reads=["p2_po"], writes=["p2_vst"])
                        em.dma("sp", I["vc_d"][:, g * 64:(g + 1) * 64].rearrange("(c p) d -> p c d", p=128), vst[:], reads=["p2_vst"], writes=["vc_d"])

    def phase3(self, layer, n_tiles=NT, n0=0, att_dst=None):
        nc, em, I = self.nc, self.em, self.I
        with Alloc(nc, em) as A:
            kselT = A.sb("p3_kselT", [128, S], BF16)
            kwinT = A.sb("p3_kwinT", [128, S], BF16)
            kcT = A.sb("p3_kcT", [128, 512], BF16)
            vselx = A.sb("p3_vselx", [128, NT, 2, 65], BF16)
            vwinx = A.sb("p3_vwinx", [128, NT, 2, 65], BF16)
            vcx = A.sb("p3_vcx", [128, 4, 2, 65], BF16)
            E = A.sb("p3_E", [128, S], BF16)
            ovl = A.sb("p3_ovl", [128, 4, 128], BF16)
            cmb = A.sb("p3_cmb", [128, 17, 128], BF16)
            idt = A.sb("p3_id", [128, 128], BF16)
            cbt = A.sb("p3_cb", [128, 128], BF16)
            wlot = A.sb("p3_wlo", [128, 128], BF16)
            anw = A.sb("p3_anw", [128, 512], F32)
            qTs = [A.sb(f"p3_qT{i}", [128, 4, 128], BF16) for i in range(2)]
            gats = [A.sb(f"p3_gat{i}", [128, 24], F32) for i in range(2)]
            bons = [A.sb(f"p3_bon{i}", [128, 128], F32) for i in range(2)]
            eTs = [A.sb(f"p3_eT{i}", [128, 512], BF16) for i in range(3)]
            rden = A.sb("p3_rden", [128, 4], F32)
            coef = A.sb("p3_coef", [128, 4], F32)
            score = A.sb("p3_score", [128, 128], F32)
            work = A.sb("p3_work", [128, 128], F32)
            m8 = A.sb("p3_m8", [128, 8], F32)
            m8b = A.sb("p3_m8b", [128, 8], F32)
            self_f = A.sb("p3_self", [128, 128], F32)
            selb = A.sb("p3_selb", [128, 128], BF16)
            selmT = A.sb("p3_selmT", [128, 128], BF16)
            tmp = A.sb("p3_tmp", [128, 4, 64], F32)
            att = A.sb("p3_att", [128, 512], F32)
            junk = A.sb("p3_junk", [128, 512], F32)
            attb = A.sb("p3_attb", [128, 512], BF16)
            ss = A.sb("p3_ss", [128, 1], F32)
            Sb = [A.ps(f"p3_S{i}", [128, 512], F32) for i in range(3)]
            OC = A.ps("p3_OC", [128, 512], F32)
            IMP = A.ps("p3_IMP", [128, 512], F32)
            OS = A.ps("p3_OS", [128, 512], F32)
            OW = A.ps("p3_OW", [128, 512], F32)
            TP = A.ps("p3_TP", [128, 1024], BF16)
            for j in range(4):
                cs = slice(j * 2048, (j + 1) * 2048)
                em.dma("sp", kselT[:, cs], I["kselT_d"][:, cs], reads=["kselT_d"], writes=["p3_kselT"])
                em.dma("act", kwinT[:, cs], I["kwinT_d"][:, cs], reads=["kwinT_d"], writes=["p3_kwinT"])
                em.dma("sp", E[:, cs], I["E_all"][:, cs], writes=["p3_E"])
            em.dma("act", kcT[:], I["kcT_d"], reads=["kcT_d"], writes=["p3_kcT"])
            for g in range(2):
                for j in range(4):
                    ks = slice(j * 16, (j + 1) * 16)
                    rs_ = slice(j * 2048, (j + 1) * 2048)
                    em.dma("sp", vselx[:, ks, g, 0:64], I["vsel_d"][rs_, g * 64:(g + 1) * 64].rearrange("(k p) d -> p k d", p=128), reads=["vsel_d"], writes=["p3_vselx"])
                    em.dma("act", vwinx[:, ks, g, 0:64], I["vwin_d"][rs_, g * 64:(g + 1) * 64].rearrange("(k p) d -> p k d", p=128), reads=["vwin_d"], writes=["p3_vwinx"])
                em.dma("sp", vcx[:, :, g, 0:64], I["vc_d"][:, g * 64:(g + 1) * 64].rearrange("(k p) d -> p k d", p=128), reads=["vc_d"], writes=["p3_vcx"])
            em.op("dve", lambda: nc.vector.memset(vselx[:, :, :, 64:65], 1.0), writes=["p3_vselx"])
            em.op("dve", lambda: nc.vector.memset(vwinx[:, :, :, 64:65], 1.0), writes=["p3_vwinx"])
            em.op("dve", lambda: nc.vector.memset(vcx[:, :, :, 64:65], 1.0), writes=["p3_vcx"])
            em.dma("sp", ovl[:], I["overlap"].rearrange("(m p) j -> p m j", p=128), writes=["p3_ovl"])
            em.dma("act", cmb[:], I["cmaskb"].rearrange("i p q -> p i q"), writes=["p3_cmb"])
            em.dma("sp", idt[:], I["ident"], writes=["p3_id"])
            em.dma("act", cbt[:], I["cb"], writes=["p3_cb"])
            em.dma("sp", wlot[:], I["wlo"], writes=["p3_wlo"])
            em.dma("act", anw[:], row_bc(I["attn_norm_w"][layer], 512), writes=["p3_anw"])
            st = {"s": 0}

            def nextS():
                i = st["s"] % 3
                st["s"] += 1
                return Sb[i], f"p3_S{i}", eTs[i], f"p3_eT{i}"

            def b4(t):
                return bc(t, [128, 4, 128], 1)

            def s3(ps):
                return ps[:].rearrange("p (r q) -> p r q", r=4)

            def o3(ps, lo, hi):
                return ps[:, 0:260].rearrange("p (r e) -> p r e", e=65)[:, :, lo:hi]

            if att_dst is None:
                att_dst = I["att_d"]
            for n in range(n0, n_tiles):
                qT, qk = qTs[n % 2], f"p3_qT{n % 2}"
                gat, gk = gats[n % 2], f"p3_gat{n % 2}"
                bon, bk = bons[n % 2], f"p3_bon{n % 2}"
                cols = slice(n * 128, (n + 1) * 128)
                em.dma("sp", qT[:], I["qT_d"][:, :, cols].rearrange("r p q -> p r q"), reads=["qT_d"], writes=[qk])
                em.dma("act", gat[:], I["gates_d"][cols, :], reads=["gates_d"], writes=[gk])
                em.dma("act", bon[:], I["bonus"][n], writes=[bk])
                for g in range(2):
                    ps_ = slice(64 * g, 64 * (g + 1))
                    qg = qT[ps_, :, :]
                    n_ct = (8 * n + 6) // 128 + 1
                    for m in range(n_ct):
                        S_, sk, eT, ek = nextS()
                        need = n < 16 * m + 17
                        em.op("pe", lambda S_=S_, m=m, need=need: nc.tensor.matmul(s3(S_), lhsT=kcT[ps_, m * 128:(m + 1) * 128], rhs=qg, start=True, stop=not need),
                              reads=["p3_kcT", qk], writes=[sk])
                        if need:
                            pi = n - 16 * m
                            em.op("pe", lambda S_=S_, pi=pi: nc.tensor.matmul(s3(S_), lhsT=idt[:], rhs=b4(cmb[:, pi, :]), start=False, stop=True),
                                  reads=["p3_id", "p3_cmb"], writes=[sk])
                        em.op("act", lambda S_=S_, eT=eT: nc.scalar.activation(out=eT[:], in_=S_[:], func=AF.Exp), reads=[sk], writes=[ek])
                        for r in range(4):
                            em.op("pe", lambda r=r, m=m, eT=eT: nc.tensor.matmul(OC[:, r * 65:(r + 1) * 65], lhsT=eT[:, r * 128:(r + 1) * 128], rhs=vcx[:, m, g, :],
                                                                               start=(m == 0 and r == 0), stop=(m == n_ct - 1 and r == 3), skip_group_check=True),
                                  reads=[ek, "p3_vcx"], writes=["p3_OC"])
                        for r in range(4):
                            em.op("pe", lambda r=r, m=m, eT=eT: nc.tensor.matmul(IMP[:, r * 128:(r + 1) * 128], lhsT=eT[:, r * 128:(r + 1) * 128], rhs=ovl[:, m, :],
                                                                               start=(m == 0 and r == 0), stop=(m == n_ct - 1 and r == 3), skip_group_check=True),
                                  reads=[ek, "p3_ovl"], writes=["p3_IMP"])
                    em.op("dve", lambda: nc.vector.tensor_scalar(out=rden[:].unsqueeze(2), in0=o3(OC, 64, 65), scalar1=1e-30, scalar2=None, op0=ALU.max), reads=["p3_OC"], writes=["p3_rden"])
                    em.op("dve", lambda: nc.vector.reciprocal(out=rden[:], in_=rden[:]), reads=["p3_rden"], writes=["p3_rden"])
                    em.op("dve", lambda: nc.vector.scalar_tensor_tensor(out=score[:], in0=IMP[:, 0:128], scalar=rden[:, 0:1], in1=bon[:], op0=ALU.mult, op1=ALU.add),
                          reads=["p3_IMP", "p3_rden", bk], writes=["p3_score"])
                    for r in range(1, 4):
                        em.op("dve", lambda r=r: nc.vector.scalar_tensor_tensor(out=score[:], in0=IMP[:, r * 128:(r + 1) * 128], scalar=rden[:, r:r + 1], in1=score[:], op0=ALU.mult, op1=ALU.add),
                              reads=["p3_IMP", "p3_rden", "p3_score"], writes=["p3_score"])
                    gv = gat[:, g * 12:(g + 1) * 12].rearrange("p (r b) -> p r b", b=3)
                    em.op("dve", lambda gv=gv: nc.vector.tensor_tensor(out=coef[:].unsqueeze(2), in0=rden[:].unsqueeze(2), in1=gv[:, :, 0:1], op=ALU.mult), reads=["p3_rden", gk], writes=["p3_coef"])
                    attg = att[:, g * 256:(g + 1) * 256].rearrange("p (r d) -> p r d", d=64)
                    em.op("dve", lambda attg=attg: nc.vector.tensor_tensor(out=attg, in0=o3(OC, 0, 64), in1=bc(coef[:], [128, 4, 64], 2), op=ALU.mult), reads=["p3_OC", "p3_coef"], writes=["p3_att"])
                    em.op("dve", lambda: nc.vector.max(out=m8[:], in_=score[:]), reads=["p3_score"], writes=["p3_m8"])
                    em.op("dve", lambda: nc.vector.match_replace(out=work[:], in_to_replace=m8[:], in_values=score[:], imm_value=-3.0e38), reads=["p3_m8", "p3_score"], writes=["p3_work"])
                    em.op("dve", lambda: nc.vector.max(out=m8b[:], in_=work[:]), reads=["p3_work"], writes=["p3_m8b"])
                    em.op("dve", lambda: nc.vector.tensor_scalar(out=self_f[:], in0=score[:], scalar1=m8b[:, 7:8], scalar2=1.0, op0=ALU.is_ge, op1=ALU.subtract), reads=["p3_score", "p3_m8b"], writes=["p3_self"])
                    em.op("dve", lambda: nc.vector.tensor_scalar(out=selb[:], in0=self_f[:], scalar1=-NEGB, scalar2=None, op0=ALU.mult), reads=["p3_self"], writes=["p3_selb"])
                    em.op("pe", lambda: nc.tensor.transpose(out=TP[:, 0:128], in_=selb[:], identity=idt[:]), reads=["p3_selb", "p3_id"], writes=["p3_TP"])
                    em.op("act", lambda: nc.scalar.copy(out=selmT[:], in_=TP[:, 0:128]), reads=["p3_TP"], writes=["p3_selmT"])
                    for (br, kT, kk, vx, vk, O_, ok_, k0) in ((2, kwinT, "p3_kwinT", vwinx, "p3_vwinx", OW, "p3_OW", max(0, n - 4)), (1, kselT, "p3_kselT", vselx, "p3_vselx", OS, "p3_OS", 0)):
                        pend = None

                        def emit_pv(pd, O_=O_, ok_=ok_, vx=vx, vk=vk, k0=k0):
                            kt_, eT_, ek_ = pd
                            for r in range(4):
                                em.op("pe", lambda r=r: nc.tensor.matmul(O_[:, r * 65:(r + 1) * 65], lhsT=eT_[:, r * 128:(r + 1) * 128], rhs=vx[:, kt_, g, :],
                                                                         start=(kt_ == k0 and r == 0), stop=(kt_ == n and r == 3), skip_group_check=True),
                                      reads=[ek_, vk], writes=[ok_])

                        for kt in range(k0, n + 1):
                            S_, sk, eT, ek = nextS()
                            extra = []
                            if br == 1:
                                extra.append((E[:, kt * 128:(kt + 1) * 128], "p3_E", selmT, "p3_selmT"))
                            if kt == n:
                                extra.append((idt[:], "p3_id", cbt, "p3_cb"))
                            if br == 2 and kt == n - 4:
                                extra.append((idt[:], "p3_id", wlot, "p3_wlo"))
                            em.op("pe", lambda S_=S_, kt=kt, kT=kT, extra=extra: nc.tensor.matmul(s3(S_), lhsT=kT[ps_, kt * 128:(kt + 1) * 128], rhs=qg, start=True, stop=(len(extra) == 0)),
                                  reads=[kk, qk], writes=[sk])
                            for xi, (lh, lk, rt, rk) in enumerate(extra):
                                em.op("pe", lambda S_=S_, lh=lh, rt=rt, xi=xi, extra=extra: nc.tensor.matmul(s3(S_), lhsT=lh, rhs=b4(rt[:]), start=False, stop=(xi == len(extra) - 1)),
                                      reads=[lk, rk], writes=[sk])
                            em.op("act", lambda S_=S_, eT=eT: nc.scalar.activation(out=eT[:], in_=S_[:], func=AF.Exp), reads=[sk], writes=[ek])
                            if pend is not None:
                                emit_pv(pend)
                            pend = (kt, eT, ek)
                        emit_pv(pend)
                        em.op("dve", lambda O_=O_: nc.vector.tensor_scalar(out=rden[:].unsqueeze(2), in0=o3(O_, 64, 65), scalar1=1e-30, scalar2=None, op0=ALU.max), reads=[ok_], writes=["p3_rden"])
                        em.op("dve", lambda: nc.vector.reciprocal(out=rden[:], in_=rden[:]), reads=["p3_rden"], writes=["p3_rden"])
                        em.op("dve", lambda gv=gv, br=br: nc.vector.tensor_tensor(out=coef[:].unsqueeze(2), in0=rden[:].unsqueeze(2), in1=gv[:, :, br:br + 1], op=ALU.mult), reads=["p3_rden", gk], writes=["p3_coef"])
                        em.op("dve", lambda O_=O_: nc.vector.tensor_tensor(out=tmp[:], in0=o3(O_, 0, 64), in1=bc(coef[:], [128, 4, 64], 2), op=ALU.mult), reads=[ok_, "p3_coef"], writes=["p3_tmp"])
                        em.op("pool", lambda attg=attg: nc.gpsimd.tensor_tensor(out=attg, in0=attg, in1=tmp[:], op=ALU.add), reads=["p3_tmp", "p3_att"], writes=["p3_att"])
                em.op("act", lambda: nc.scalar.activation(out=junk[:], in_=att[:], func=AF.Square, accum_out=ss[:]), reads=["p3_att"], writes=["p3_junk", "p3_ss"])
                em.op("act", lambda: nc.scalar.activation(out=ss[:], in_=ss[:], func=AF.Ln, scale=1.0 / 512, bias=RMS_EPS), reads=["p3_ss"], writes=["p3_ss"])
                em.op("act", lambda: nc.scalar.activation(out=ss[:], in_=ss[:], func=AF.Exp, scale=-0.5), reads=["p3_ss"], writes=["p3_ss"])
                em.op("dve", lambda: nc.vector.scalar_tensor_tensor(out=attb[:], in0=att[:], scalar=ss[:, 0:1], in1=anw[:], op0=ALU.mult, op1=ALU.mult), reads=["p3_att", "p3_ss", "p3_anw"], writes=["p3_attb"])
                em.dma("sp", att_dst[(n - n0) * 128:(n - n0 + 1) * 128, :], attb[:], reads=["p3_attb"], writes=["att_d"])


    def phase4(self, layer, n_chunks=NT):
        nc, em, I = self.nc, self.em, self.I
        with Alloc(nc, em) as A:
            xin = A.sb("p4_xin", [128, S + 3], BF16)
            cw = A.sb("p4_cw", [128, 8, 4], F32)
            cbs = A.sb("p4_cbs", [128, 8], F32)
            accs = [A.sb(f"p4_acc{i}", [128, 2048], F32) for i in range(2)]
            outs = [A.sb(f"p4_out{i}", [128, 2048], BF16) for i in range(2)]
            em.dma("sp", cw[:], I["conv_w_r"][layer], writes=["p4_cw"])
            em.dma("act", cbs[:], I["conv_b_r"][layer], writes=["p4_cbs"])
            em.op("dve", lambda: nc.vector.memset(xin[:, 0:3], 0.0), writes=["p4_xin"])
            it = 0
            for ch in range(8):
                for j in range(4):
                    em.dma(("sp", "act")[j % 2], xin[:, 3 + j * 2048:3 + (j + 1) * 2048], I["xbcT_d"][ch, :, j * 2048:(j + 1) * 2048], reads=["xbcT_d"], writes=["p4_xin"])
                for j in range(4):
                    acc, ak = accs[it % 2], f"p4_acc{it % 2}"
                    ot, okk = outs[it % 2], f"p4_out{it % 2}"
                    it += 1
                    c0 = j * 2048
                    em.op("dve", lambda acc=acc, c0=c0, ch=ch: nc.vector.tensor_scalar(out=acc[:], in0=xin[:, c0:c0 + 2048], scalar1=cw[:, ch, 0:1], scalar2=cbs[:, ch:ch + 1], op0=ALU.mult, op1=ALU.add),
                          reads=["p4_xin", "p4_cw", "p4_cbs"], writes=[ak])
                    for k in range(1, 4):
                        em.op("dve", lambda acc=acc, c0=c0, ch=ch, k=k: nc.vector.scalar_tensor_tensor(out=acc[:], in0=xin[:, c0 + k:c0 + k + 2048], scalar=cw[:, ch, k:k + 1], in1=acc[:], op0=ALU.mult, op1=ALU.add),
                              reads=["p4_xin", "p4_cw", ak], writes=[ak])
                    em.op("act", lambda acc=acc, ot=ot: nc.scalar.activation(out=ot[:], in_=acc[:], func=AF.Silu), reads=[ak], writes=[okk])
                    em.dma(("sp", "act")[j % 2], I["xbc2T_d"][ch, :, c0:c0 + 2048], ot[:], reads=[okk], writes=["xbc2T_d"])
        with Alloc(nc, em) as A:
            idt = A.sb("p4_id", [128, 128], BF16)
            tri = A.sb("p4_tri", [128, 128], F32)
            tris = A.sb("p4_tris", [128, 128], F32)
            ones = A.sb("p4_ones", [128, 128], F32)
            abc = A.sb("p4_abc", [128, 8], F32)
            dsk = A.sb("p4_dsk", [128, 8], F32)
            snw = A.sb("p4_snw", [128, 512], F32)
            xts = [A.sb(f"p4_xt{i}", [128, 8, 128], BF16) for i in range(2)]
            dts = [A.sb(f"p4_dt{i}", [128, 8], F32) for i in range(2)]
            zts = [A.sb(f"p4_z{i}", [128, 512], BF16) for i in range(2)]
            xb_2 = [A.sb(f"p4_xb{i}", [128, 768], BF16) for i in range(2)]
            dta_2 = [A.sb(f"p4_dta{i}", [128, 8], F32) for i in range(2)]
            sa_2 = [A.sb(f"p4_sa{i}", [128, 16], F32) for i in range(2)]
            eac_2 = [A.sb(f"p4_eac{i}", [128, 8], F32) for i in range(2)]
            te_2 = [A.sb(f"p4_te{i}", [128, 8], F32) for i in range(2)]
            el_2 = [A.sb(f"p4_el{i}", [128, 8], F32) for i in range(2)]
            xdt_2 = [A.sb(f"p4_xdt{i}", [128, 8, 64], BF16) for i in range(2)]
            xdte_2 = [A.sb(f"p4_xdte{i}", [128, 8, 64], BF16) for i in range(2)]
            scm_2 = [A.sb(f"p4_scm{i}", [128, 2, 128], F32) for i in range(2)]
            Rs = [A.sb(f"p4_R{i}", [128, 128], F32) for i in range(2)]
            Wd_2 = [A.sb(f"p4_Wd{i}", [128, 8, 128], F32) for i in range(2)]
            W_2 = [A.sb(f"p4_W{i}", [128, 8, 128], BF16) for i in range(2)]
            stf = A.sb("p4_stf", [128, 8, 64], F32)
            stb = A.sb("p4_stb", [128, 8, 64], BF16)
            ysb_2 = [A.sb(f"p4_ysb{i}", [128, 512], F32) for i in range(2)]
            yi_2 = [A.sb(f"p4_yi{i}", [128, 512], F32) for i in range(2)]
            szt_2 = [A.sb(f"p4_szt{i}", [128, 512], F32) for i in range(2)]
            junk_2 = [A.sb(f"p4_junk{i}", [128, 256], F32) for i in range(2)]
            ss2_2 = [A.sb(f"p4_ss2{i}", [128, 2], F32) for i in range(2)]
            yo_2 = [A.sb(f"p4_yo{i}", [128, 512], BF16) for i in range(2)]
            TP = A.ps("p4_TP", [128, 1024], BF16)
            PA = A.ps("p4_PA", [128, 512], F32)
            PSc = A.ps("p4_PSc", [128, 512], F32)
            PD = [A.ps(f"p4_PD{i}", [128, 512], F32) for i in range(2)]
            PY = A.ps("p4_PY", [128, 512], F32)
            PI = A.ps("p4_PI", [128, 512], F32)
            PSt = A.ps("p4_PSt", [128, 512], F32)
            em.dma("sp", idt[:], I["ident"], writes=["p4_id"])
            em.dma("act", tri[:], I["tri"], writes=["p4_tri"])
            em.dma("sp", tris[:], I["tris"], writes=["p4_tris"])
            em.dma("act", ones[:], I["ones"], writes=["p4_ones"])
            em.dma("sp", abc[:], row_bc(I["a_log"][layer], 8), writes=["p4_abc"])
            em.dma("act", dsk[:], row_bc(I["d_skip"][layer], 8), writes=["p4_dsk"])
            em.dma("sp", snw[:], row_bc(I["ssm_norm_w"][layer], 512), writes=["p4_snw"])
            em.op("act", lambda: nc.scalar.activation(out=abc[:], in_=abc[:], func=AF.Exp), reads=["p4_abc"], writes=["p4_abc"])
            em.op("dve", lambda: nc.vector.tensor_scalar(out=abc[:], in0=abc[:], scalar1=-1.0, scalar2=None, op0=ALU.mult), reads=["p4_abc"], writes=["p4_abc"])
            em.op("dve", lambda: nc.vector.memset(stf[:], 0.0), writes=["p4_stf"])
            em.op("dve", lambda: nc.vector.memset(stb[:], 0.0), writes=["p4_stb"])
            ri = 0
            for c in range(n_chunks):
                p = c % 2
                xb = xb_2[p]
                dta = dta_2[p]
                sa = sa_2[p]
                eac = eac_2[p]
                te = te_2[p]
                el = el_2[p]
                xdt = xdt_2[p]
                xdte = xdte_2[p]
                scm = scm_2[p]
                Wd = Wd_2[p]
                W = W_2[p]
                ysb = ysb_2[p]
                yi = yi_2[p]
                szt = szt_2[p]
                junk = junk_2[p]
                ss2 = ss2_2[p]
                yo = yo_2[p]
                xt, xk = xts[c % 2], f"p4_xt{c % 2}"
                dtt, dk = dts[c % 2], f"p4_dt{c % 2}"
                zt, zk = zts[c % 2], f"p4_z{c % 2}"
                cols = slice(c * 128, (c + 1) * 128)
                em.dma("sp", xt[:], I["xbc2T_d"][:, :, cols].rearrange("c p t -> p c t"), reads=["xbc2T_d"], writes=[xk])
                em.dma("act", dtt[:], I["dt_d"][cols, :], reads=["dt_d"], writes=[dk])
                em.dma("act", zt[:], I["z_d"][cols, :], reads=["z_d"], writes=[zk])
                for k in range(6):
                    em.op("pe", lambda k=k, xt=xt: nc.tensor.transpose(out=TP[:, k * 128:(k + 1) * 128], in_=xt[:, k, :], identity=idt[:]), reads=[xk, "p4_id"], writes=["p4_TP"])
                em.op("act", lambda: nc.scalar.copy(out=xb[:], in_=TP[:, 0:768]), reads=["p4_TP"], writes=[f"p4_xb{p}"])
                em.op("dve", lambda dtt=dtt: nc.vector.tensor_tensor(out=dta[:], in0=dtt[:], in1=abc[:], op=ALU.mult), reads=[dk, "p4_abc"], writes=[f"p4_dta{p}"])
                em.op("pe", lambda: nc.tensor.matmul(PA[:, 0:8], lhsT=tri[:], rhs=dta[:], start=True, stop=True), reads=["p4_tri", f"p4_dta{p}"], writes=["p4_PA"])
                em.op("pe", lambda: nc.tensor.matmul(PA[:, 8:16], lhsT=ones[:], rhs=dta[:], start=True, stop=True), reads=["p4_ones", f"p4_dta{p}"], writes=["p4_PA"])
                em.op("act", lambda: nc.scalar.copy(out=sa[:], in_=PA[:, 0:16]), reads=["p4_PA"], writes=[f"p4_sa{p}"])
                em.op("act", lambda: nc.scalar.activation(out=eac[:], in_=sa[:, 0:8], func=AF.Exp), reads=[f"p4_sa{p}"], writes=[f"p4_eac{p}"])
                em.op("dve", lambda: nc.vector.tensor_tensor(out=te[:], in0=sa[:, 8:16], in1=sa[:, 0:8], op=ALU.subtract), reads=[f"p4_sa{p}"], writes=[f"p4_te{p}"])
                em.op("act", lambda: nc.scalar.activation(out=te[:], in_=te[:], func=AF.Exp), reads=[f"p4_te{p}"], writes=[f"p4_te{p}"])
                em.op("act", lambda: nc.scalar.activation(out=el[:], in_=sa[:, 8:16], func=AF.Exp), reads=[f"p4_sa{p}"], writes=[f"p4_el{p}"])
                xs3 = xb[:, 0:512].rearrange("p (h d) -> p h d", d=64)
                em.op("dve", lambda dtt=dtt, xs3=xs3: nc.vector.tensor_tensor(out=xdt[:], in0=xs3, in1=bc(dtt[:], [128, 8, 64], 2), op=ALU.mult), reads=[f"p4_xb{p}", dk], writes=[f"p4_xdt{p}"])
                em.op("dve", lambda: nc.vector.tensor_tensor(out=xdte[:], in0=xdt[:], in1=bc(te[:], [128, 8, 64], 2), op=ALU.mult), reads=[f"p4_xdt{p}", f"p4_te{p}"], writes=[f"p4_xdte{p}"])
                for g in range(2):
                    em.op("pe", lambda g=g, xt=xt: nc.tensor.matmul(PSc[:, g * 128:(g + 1) * 128], lhsT=xt[:, 4 + g, :], rhs=xt[:, 6 + g, :], start=True, stop=True), reads=[xk], writes=["p4_PSc"])
                em.op("dve", lambda: nc.vector.tensor_tensor(out=scm[:], in0=PSc[:, 0:256].rearrange("p (g i) -> p g i", g=2), in1=bc(tri[:], [128, 2, 128], 1), op=ALU.mult),
                      reads=["p4_PSc", "p4_tri"], writes=[f"p4_scm{p}"])
                for h in range(8):
                    R, rk = Rs[ri % 2], f"p4_R{ri % 2}"
                    ri += 1
                    em.op("pool", lambda R=R, h=h: nc.gpsimd.tensor_scalar(out=R[:], in0=tri[:], scalar1=dta[:, h:h + 1], scalar2=None, op0=ALU.mult), reads=["p4_tri", f"p4_dta{p}"], writes=[rk])
                    em.op("pe", lambda R=R, h=h: nc.tensor.matmul(PD[h // 4][:, (h % 4) * 128:(h % 4 + 1) * 128], lhsT=tris[:], rhs=R[:], start=True, stop=True),
                          reads=["p4_tris", rk], writes=[f"p4_PD{h // 4}"])
                for g in range(2):
                    em.op("act", lambda g=g: nc.scalar.activation(out=Wd[:, g * 4:(g + 1) * 4, :], in_=PD[g][:].rearrange("p (h i) -> p h i", h=4), func=AF.Exp), reads=[f"p4_PD{g}"], writes=[f"p4_Wd{p}"])
                    em.op("dve", lambda g=g: nc.vector.tensor_tensor(out=W[:, g * 4:(g + 1) * 4, :], in0=Wd[:, g * 4:(g + 1) * 4, :], in1=bc(scm[:, g, :], [128, 4, 128], 1), op=ALU.mult),
                          reads=[f"p4_Wd{p}", f"p4_scm{p}"], writes=[f"p4_W{p}"])
                for h in range(8):
                    g = h // 4
                    hs = slice(h * 64, (h + 1) * 64)
                    em.op("pe", lambda h=h, hs=hs: nc.tensor.matmul(PY[:, hs], lhsT=W[:, h, :], rhs=xdt[:, h, :], start=True, stop=True), reads=[f"p4_W{p}", f"p4_xdt{p}"], writes=["p4_PY"])
                    em.op("pe", lambda h=h, hs=hs, g=g, xt=xt: nc.tensor.matmul(PI[:, hs], lhsT=xt[:, 6 + g, :], rhs=stb[:, h, :], start=True, stop=True), reads=[xk, "p4_stb"], writes=["p4_PI"])
                    em.op("pe", lambda h=h, hs=hs, g=g: nc.tensor.matmul(PSt[:, hs], lhsT=xb[:, 512 + g * 128:512 + (g + 1) * 128], rhs=xdte[:, h, :], start=True, stop=True), reads=[f"p4_xb{p}", f"p4_xdte{p}"], writes=["p4_PSt"])
                y3 = lambda t: t[:].rearrange("p (h d) -> p h d", d=64)
                em.op("act", lambda: nc.scalar.copy(out=ysb[:], in_=PY[:]), reads=["p4_PY"], writes=[f"p4_ysb{p}"])
                em.op("dve", lambda: nc.vector.tensor_tensor(out=y3(yi), in0=y3(PI), in1=bc(eac[:], [128, 8, 64], 2), op=ALU.mult), reads=["p4_PI", f"p4_eac{p}"], writes=[f"p4_yi{p}"])
                em.op("pool", lambda: nc.gpsimd.tensor_tensor(out=ysb[:], in0=ysb[:], in1=yi[:], op=ALU.add), reads=[f"p4_ysb{p}", f"p4_yi{p}"], writes=[f"p4_ysb{p}"])
                em.op("dve", lambda xs3=xs3: nc.vector.tensor_tensor(out=y3(yi), in0=xs3, in1=bc(dsk[:], [128, 8, 64], 2), op=ALU.mult), reads=[f"p4_xb{p}", "p4_dsk", f"p4_ysb{p}"], writes=[f"p4_yi{p}"])
                em.op("pool", lambda: nc.gpsimd.tensor_tensor(out=ysb[:], in0=ysb[:], in1=yi[:], op=ALU.add), reads=[f"p4_ysb{p}", f"p4_yi{p}"], writes=[f"p4_ysb{p}"])
                em.op("dve", lambda: nc.vector.tensor_tensor(out=stf[:], in0=stf[:], in1=bc(el[:], [128, 8, 64], 2), op=ALU.mult), reads=["p4_stf", f"p4_el{p}"], writes=["p4_stf"])
                em.op("dve", lambda: nc.vector.tensor_tensor(out=stf[:], in0=stf[:], in1=y3(PSt), op=ALU.add), reads=["p4_stf", "p4_PSt"], writes=["p4_stf"])
                em.op("act", lambda: nc.scalar.copy(out=stb[:], in_=stf[:]), reads=["p4_stf"], writes=["p4_stb"])
                em.op("act", lambda zt=zt: nc.scalar.activation(out=szt[:], in_=zt[:], func=AF.Silu), reads=[zk], writes=[f"p4_szt{p}"])
                em.op("dve", lambda: nc.vector.tensor_tensor(out=ysb[:], in0=ysb[:], in1=szt[:], op=ALU.mult), reads=[f"p4_ysb{p}", f"p4_szt{p}"], writes=[f"p4_ysb{p}"])
                for g in range(2):
                    em.op("act", lambda g=g: nc.scalar.activation(out=junk[:], in_=ysb[:, g * 256:(g + 1) * 256], func=AF.Square, accum_out=ss2[:, g:g + 1]), reads=[f"p4_ysb{p}"], writes=[f"p4_junk{p}", f"p4_ss2{p}"])
                em.op("act", lambda: nc.scalar.activation(out=ss2[:], in_=ss2[:], func=AF.Ln, scale=1.0 / 256, bias=RMS_EPS), reads=[f"p4_ss2{p}"], writes=[f"p4_ss2{p}"])
                em.op("act", lambda: nc.scalar.activation(out=ss2[:], in_=ss2[:], func=AF.Exp, scale=-0.5), reads=[f"p4_ss2{p}"], writes=[f"p4_ss2{p}"])
                for g in range(2):
                    gs = slice(g * 256, (g + 1) * 256)
                    em.op("dve", lambda g=g, gs=gs: nc.vector.scalar_tensor_tensor(out=yo[:, gs], in0=ysb[:, gs], scalar=ss2[:, g:g + 1], in1=snw[:, gs], op0=ALU.mult, op1=ALU.mult),
                          reads=[f"p4_ysb{p}", f"p4_ss2{p}", "p4_snw"], writes=[f"p4_yo{p}"])
                em.dma("sp", I["ssm_d"][cols, :], yo[:], reads=[f"p4_yo{p}"], writes=["ssm_d"])


    def phase5(self, layer, x_src, mod, n_tiles=NT, att_src=None):
        nc, em, I = self.nc, self.em, self.I
        with Alloc(nc, em) as A:
            wo = A.sb("p5_wo", [128, 8, D], BF16)
            wss = [A.sb(f"p5_ws{i}", [128, D], F32) for i in range(2)]
            l1w = A.sb("p5_l1w", [128, D], F32)
            l1b = A.sb("p5_l1b", [128, D], F32)
            idt = A.sb("p5_id", [128, 128], BF16)
            cats = [A.sb(f"p5_cat{i}", [128, D], BF16) for i in range(2)]
            xs = [A.sb(f"p5_x{i}", [128, D], F32) for i in range(2)]
            catT = A.sb("p5_catT", [128, 8, 128], BF16)
            t = A.sb("p5_t", [128, D], F32)
            xn = A.sb("p5_xn", [128, D], F32)
            x1t = A.sb("p5_x1t", [128, D], F32)
            ub = A.sb("p5_ub", [128, D], BF16)
            u2s = A.sb("p5_u2s", [128, 8, 128], BF16)
            st = A.sb("p5_st", [128, 2, 6], F32)
            mv = A.sb("p5_mv", [128, 2], F32)
            rs = A.sb("p5_rs", [128, 1], F32)
            TP = A.ps("p5_TP", [128, 1024], BF16)
            TP2 = A.ps("p5_TP2", [128, 1024], BF16)
            H = [A.ps(f"p5_H{i}", [128, 512], F32) for i in range(2)]
            em.dma("sp", idt[:], I["ident"], writes=["p5_id"])
            em.dma("act", l1w[:], row_bc(I["ln1_w"][layer], D), writes=["p5_l1w"])
            em.dma("sp", l1b[:], row_bc(I["ln1_b"][layer], D), writes=["p5_l1b"])
            for k in range(8):
                em.dma(("sp", "act")[k % 2], wss[k % 2][:], I["w_out"][layer, k * 128:(k + 1) * 128, :], writes=[f"p5_ws{k % 2}"])
                em.op("pool", lambda k=k: nc.gpsimd.tensor_copy(out=wo[:, k, :], in_=wss[k % 2][:]), reads=[f"p5_ws{k % 2}"], writes=["p5_wo"])
            for n in range(n_tiles):
                cat, ck = cats[n % 2], f"p5_cat{n % 2}"
                xt, xk = xs[n % 2], f"p5_x{n % 2}"
                rows = slice(n * 128, (n + 1) * 128)
                a_ap = att_src(n) if att_src is not None else I["att_d"][rows, :]
                em.dma("sp", cat[:, 0:512], a_ap, reads=["att_d"], writes=[ck])
                em.dma("sp", cat[:, 512:1024], I["ssm_d"][rows, :], reads=["ssm_d"], writes=[ck])
                em.dma("act", xt[:], x_src[rows, :], reads=["xsrc"], writes=[xk])
                for k in range(8):
                    em.op("pe", lambda k=k, cat=cat: nc.tensor.transpose(out=TP[:, k * 128:(k + 1) * 128], in_=cat[:, k * 128:(k + 1) * 128], identity=idt[:]), reads=[ck, "p5_id"], writes=["p5_TP"])
                em.op("act", lambda: nc.scalar.copy(out=catT[:], in_=TP[:].rearrange("p (k t) -> p k t", t=128)), reads=["p5_TP"], writes=["p5_catT"])
                for hf in range(2):
                    for k in range(8):
                        em.op("pe", lambda k=k, hf=hf: nc.tensor.matmul(H[hf][:], lhsT=catT[:, k, :], rhs=wo[:, k, hf * 512:(hf + 1) * 512], start=(k == 0), stop=(k == 7)),
                              reads=["p5_catT", "p5_wo"], writes=[f"p5_H{hf}"])
                    em.op("dve", lambda hf=hf: nc.vector.tensor_tensor(out=t[:, hf * 512:(hf + 1) * 512], in0=H[hf][:], in1=mod[:, 2048 + hf * 512:2048 + (hf + 1) * 512], op=ALU.mult),
                          reads=[f"p5_H{hf}", "mod"], writes=["p5_t"])
                em.op("dve", lambda xt=xt: nc.vector.scalar_tensor_tensor(out=t[:], in0=xt[:], scalar=DN_ALPHA, in1=t[:], op0=ALU.mult, op1=ALU.add), reads=[xk, "p5_t"], writes=["p5_t"])
                self.ln_stats(t, "p5_t", st, mv, rs, "p5_")
                em.op("act", lambda: nc.scalar.activation(out=xn[:], in_=t[:], func=AF.Identity, scale=rs[:, 0:1], bias=mv[:, 1:2]), reads=["p5_t", "p5_mv", "p5_rs"], writes=["p5_xn"])
                em.op("dve", lambda: nc.vector.tensor_tensor(out=xn[:], in0=xn[:], in1=l1w[:], op=ALU.mult), reads=["p5_xn", "p5_l1w"], writes=["p5_xn"])
                em.op("pool", lambda: nc.gpsimd.tensor_tensor(out=x1t[:], in0=xn[:], in1=l1b[:], op=ALU.add), reads=["p5_xn", "p5_l1b"], writes=["p5_x1t"])
                em.dma("sp", I["x1_d"][rows, :], x1t[:], reads=["p5_x1t"], writes=["x1_d"])
                self.ln_stats(x1t, "p5_x1t", st, mv, rs, "p5_")
                em.op("act", lambda: nc.scalar.activation(out=xn[:], in_=x1t[:], func=AF.Identity, scale=rs[:, 0:1], bias=mv[:, 1:2]), reads=["p5_x1t", "p5_mv", "p5_rs"], writes=["p5_xn"])
                em.op("dve", lambda: nc.vector.tensor_tensor(out=xn[:], in0=xn[:], in1=mod[:, 4096:5120], op=ALU.mult), reads=["p5_xn", "mod"], writes=["p5_xn"])
                em.op("pool", lambda: nc.gpsimd.tensor_tensor(out=ub[:], in0=xn[:], in1=mod[:, 3072:4096], op=ALU.add), reads=["p5_xn", "mod"], writes=["p5_ub"])
                for k in range(8):
                    em.op("pe", lambda k=k: nc.tensor.transpose(out=TP2[:, k * 128:(k + 1) * 128], in_=ub[:, k * 128:(k + 1) * 128], identity=idt[:]), reads=["p5_ub", "p5_id"], writes=["p5_TP2"])
                em.op("act", lambda: nc.scalar.copy(out=u2s[:], in_=TP2[:].rearrange("p (k t) -> p k t", t=128)), reads=["p5_TP2"], writes=["p5_u2s"])
                em.dma("act", I["u2T_d"][:, :, rows].rearrange("c p t -> p c t"), u2s[:], reads=["p5_u2s"], writes=["u2T_d"])

    def phase6(self, layer, mod, moe, dst, n_tt=8, is_output=False, t0=0):
        nc, em, I = self.nc, self.em, self.I
        NE = NEXP if moe else 1
        F = FFN_EXPERT if moe else FFN_DENSE
        NFG = F // 256
        with Alloc(nc, em) as A:
            u2T = A.sb("p6_u2T", [128, 8, 1024], BF16)
            acc = A.sb("p6_ACC", [128, 8, D], F32)
            wgs = [A.sb(f"p6_wgs{i}", [128, 8, 256], F32) for i in range(2)]
            wus = [A.sb(f"p6_wus{i}", [128, 8, 256], F32) for i in range(2)]
            wds = [A.sb(f"p6_wds{i}", [128, 2, D], F32) for i in range(2)]
            wgb = [A.sb(f"p6_wgb{i}", [128, 8, 256], BF16) for i in range(2)]
            wub = [A.sb(f"p6_wub{i}", [128, 8, 256], BF16) for i in range(2)]
            wdb = [A.sb(f"p6_wdb{i}", [128, 2, D], BF16) for i in range(2)]
            hTs = [A.sb(f"p6_hT{i}", [128, 2, 1024], BF16) for i in range(2)]
            sgs = [A.sb(f"p6_sg{i}", [128, 512], F32) for i in range(2)]
            l2w = A.sb("p6_l2w", [128, D], F32)
            l2b = A.sb("p6_l2b", [128, D], F32)
            x1s = [A.sb(f"p6_x1{i}", [128, D], F32) for i in range(2)]
            t = A.sb("p6_t", [128, D], F32)
            xn = A.sb("p6_xn", [128, D], F32)
            ot = A.sb("p6_ot", [128, D], F32)
            st = A.sb("p6_st", [128, 2, 6], F32)
            mv = A.sb("p6_mv", [128, 2], F32)
            rs = A.sb("p6_rs", [128, 1], F32)
            G = A.sb("p6_G", [128, 8, 8], F32)
            rws = A.sb("p6_rws", [128, 8, 8], F32)
            rwb = A.sb("p6_rwb", [128, 8, 8], BF16)
            lg = A.sb("p6_lg", [128, 8], F32)
            m8 = A.sb("p6_m8", [128, 8], F32)
            nm1 = A.sb("p6_nm1", [128, 1], F32)
            e2 = A.sb("p6_e2", [128, 1], F32)
            msk = A.sb("p6_msk", [128, 8], F32)
            PG = [A.ps(f"p6_PG{i}", [128, 512], F32) for i in range(2)]
            PU = [A.ps(f"p6_PU{i}", [128, 512], F32) for i in range(2)]
            PY = [A.ps(f"p6_PY{i}", [128, 512], F32) for i in range(3)]
            PR = A.ps("p6_PR", [128, 512], F32)
            em.dma("sp", l2w[:], row_bc(I["ln2_w"][layer], D), writes=["p6_l2w"])
            em.dma("act", l2b[:], row_bc(I["ln2_b"][layer], D), writes=["p6_l2b"])
            if moe:
                em.dma("sp", rws[:], I["router_w"].rearrange("(k p) e -> p k e", p=128), writes=["p6_rws"])
                em.op("dve", lambda: nc.vector.tensor_copy(out=rwb[:], in_=rws[:]), reads=["p6_rws"], writes=["p6_rwb"])
            wi = 0
            gi = 0
            yi = 0
            for tt in range(t0, n_tt):
                tcols = slice(tt * 1024, (tt + 1) * 1024)
                for k in range(8):
                    em.dma(("sp", "act")[k % 2], u2T[:, k, :], I["u2T_d"][k, :, tcols], reads=["u2T_d"], writes=["p6_u2T"])
                if moe:
                    for sub in range(8):
                        for k in range(8):
                            em.op("pe", lambda k=k, sub=sub: nc.tensor.matmul(PR[:, 0:8], lhsT=u2T[:, k, sub * 128:(sub + 1) * 128], rhs=rwb[:, k, :], start=(k == 0), stop=(k == 7)),
                                  reads=["p6_u2T", "p6_rwb"], writes=["p6_PR"])
                        em.op("act", lambda: nc.scalar.copy(out=lg[:], in_=PR[:, 0:8]), reads=["p6_PR"], writes=["p6_lg"])
                        em.op("dve", lambda: nc.vector.max(out=m8[:], in_=lg[:]), reads=["p6_lg"], writes=["p6_m8"])
                        em.op("dve", lambda: nc.vector.tensor_scalar(out=nm1[:], in0=m8[:, 0:1], scalar1=-1.0, scalar2=None, op0=ALU.mult), reads=["p6_m8"], writes=["p6_nm1"])
                        em.op("act", lambda: nc.scalar.activation(out=e2[:], in_=m8[:, 1:2], func=AF.Exp, bias=nm1[:, 0:1]), reads=["p6_m8", "p6_nm1"], writes=["p6_e2"])
                        em.op("dve", lambda: nc.vector.tensor_scalar(out=e2[:], in0=e2[:], scalar1=1.0, scalar2=None, op0=ALU.add), reads=["p6_e2"], writes=["p6_e2"])
                        em.op("dve", lambda: nc.vector.reciprocal(out=e2[:], in_=e2[:]), reads=["p6_e2"], writes=["p6_e2"])
                        em.op("dve", lambda: nc.vector.tensor_scalar(out=msk[:], in0=lg[:], scalar1=m8[:, 1:2], scalar2=e2[:, 0:1], op0=ALU.is_ge, op1=ALU.mult), reads=["p6_lg", "p6_m8", "p6_e2"], writes=["p6_msk"])
                        em.op("act", lambda: nc.scalar.activation(out=lg[:], in_=lg[:], func=AF.Exp, bias=nm1[:, 0:1]), reads=["p6_lg", "p6_nm1"], writes=["p6_lg"])
                        em.op("dve", lambda sub=sub: nc.vector.tensor_tensor(out=G[:, sub, :], in0=lg[:], in1=msk[:], op=ALU.mult), reads=["p6_lg", "p6_msk"], writes=["p6_G"])
                first = True
                for e in range(NE):
                    wgsrc = I["exp_w_gate"][e] if moe else I["ffn_w_gate"][0]
                    wusrc = I["exp_w_up"][e] if moe else I["ffn_w_up"][0]
                    wdsrc = I["exp_w_down"][e] if moe else I["ffn_w_down"][0]
                    for fp in range(0, NFG, 2):
                        fgs = [fg for fg in (fp, fp + 1) if fg < NFG]
                        for b, fg in enumerate(fgs):
                            f0 = fg * 256
                            em.dma("sp", wgs[b][:], wgsrc[:, f0:f0 + 256].rearrange("(k p) f -> p k f", p=128), writes=[f"p6_wgs{b}"])
                            em.dma("act", wus[b][:], wusrc[:, f0:f0 + 256].rearrange("(k p) f -> p k f", p=128), writes=[f"p6_wus{b}"])
                            em.dma("sp", wds[b][:], wdsrc[f0:f0 + 256, :].rearrange("(c p) d -> p c d", p=128), writes=[f"p6_wds{b}"])
                            em.op("act", lambda b=b: nc.scalar.copy(out=wgb[b][:], in_=wgs[b][:]), reads=[f"p6_wgs{b}"], writes=[f"p6_wgb{b}"])
                            em.op("act", lambda b=b: nc.scalar.copy(out=wub[b][:], in_=wus[b][:]), reads=[f"p6_wus{b}"], writes=[f"p6_wub{b}"])
                            em.op("dve", lambda b=b: nc.vector.tensor_copy(out=wdb[b][:, 0, :], in_=wds[b][:, 0, :]), reads=[f"p6_wds{b}"], writes=[f"p6_wdb{b}"])
                            em.op("pool", lambda b=b: nc.gpsimd.tensor_copy(out=wdb[b][:, 1, :], in_=wds[b][:, 1, :]), reads=[f"p6_wds{b}"], writes=[f"p6_wdb{b}"])
                            for fc in range(2):
                                for hf in range(2):
                                    pb_ = gi % 2
                                    gi += 1
                                    for k in range(8):
                                        em.op("pe", lambda k=k, fc=fc, hf=hf, b=b, pb_=pb_: nc.tensor.matmul(PG[pb_][:], lhsT=wgb[b][:, k, fc * 128:(fc + 1) * 128], rhs=u2T[:, k, hf * 512:(hf + 1) * 512], start=(k == 0), stop=(k == 7)),
                                              reads=[f"p6_wgb{b}", "p6_u2T"], writes=[f"p6_PG{pb_}"])
                                    for k in range(8):
                                        em.op("pe", lambda k=k, fc=fc, hf=hf, b=b, pb_=pb_: nc.tensor.matmul(PU[pb_][:], lhsT=wub[b][:, k, fc * 128:(fc + 1) * 128], rhs=u2T[:, k, hf * 512:(hf + 1) * 512], start=(k == 0), stop=(k == 7)),
                                              reads=[f"p6_wub{b}", "p6_u2T"], writes=[f"p6_PU{pb_}"])
                                    em.op("act", lambda pb_=pb_: nc.scalar.activation(out=sgs[pb_][:], in_=PG[pb_][:], func=AF.Silu), reads=[f"p6_PG{pb_}"], writes=[f"p6_sg{pb_}"])
                                    em.op("dve", lambda pb_=pb_, fc=fc, hf=hf, b=b: nc.vector.tensor_tensor(out=hTs[b][:, fc, hf * 512:(hf + 1) * 512], in0=sgs[pb_][:], in1=PU[pb_][:], op=ALU.mult),
                                          reads=[f"p6_sg{pb_}", f"p6_PU{pb_}"], writes=[f"p6_hT{b}"])
                        chunks = [(b, fc) for b in range(len(fgs)) for fc in range(2)]
                        for sub in range(8):
                            for h2 in range(2):
                                yb = yi % 3
                                yi += 1
                                for ci, (b, fc) in enumerate(chunks):
                                    em.op("pe", lambda fc=fc, sub=sub, h2=h2, b=b, yb=yb, ci=ci: nc.tensor.matmul(PY[yb][:], lhsT=hTs[b][:, fc, sub * 128:(sub + 1) * 128], rhs=wdb[b][:, fc, h2 * 512:(h2 + 1) * 512], start=(ci == 0), stop=(ci == len(chunks) - 1)),
                                          reads=[f"p6_hT{b}", f"p6_wdb{b}"], writes=[f"p6_PY{yb}"])
                                dsta = acc[:, sub, h2 * 512:(h2 + 1) * 512]
                                ak = f"p6_acc{sub}_{h2}"
                                if moe:
                                    if first:
                                        em.op("dve", lambda yb=yb, sub=sub, e=e, dsta=dsta: nc.vector.tensor_scalar(out=dsta, in0=PY[yb][:], scalar1=G[:, sub, e:e + 1], scalar2=None, op0=ALU.mult),
                                              reads=[f"p6_PY{yb}", "p6_G"], writes=[ak])
                                    else:
                                        em.op("dve", lambda yb=yb, sub=sub, e=e, dsta=dsta: nc.vector.scalar_tensor_tensor(out=dsta, in0=PY[yb][:], scalar=G[:, sub, e:e + 1], in1=dsta, op0=ALU.mult, op1=ALU.add),
                                              reads=[f"p6_PY{yb}", "p6_G", ak], writes=[ak])
                                else:
                                    if first:
                                        em.op("dve", lambda yb=yb, dsta=dsta: nc.vector.tensor_copy(out=dsta, in_=PY[yb][:]), reads=[f"p6_PY{yb}"], writes=[ak])
                                    else:
                                        em.op("dve", lambda yb=yb, dsta=dsta: nc.vector.tensor_tensor(out=dsta, in0=PY[yb][:], in1=dsta, op=ALU.add), reads=[f"p6_PY{yb}", ak], writes=[ak])
                        first = False
                for sub in range(8):
                    n = tt * 8 + sub
                    rows = slice(n * 128, (n + 1) * 128)
                    x1t, xk = x1s[n % 2], f"p6_x1{n % 2}"
                    em.dma("act", x1t[:], I["x1_d"][rows, :], reads=["x1_d"], writes=[xk])
                    em.op("dve", lambda sub=sub: nc.vector.tensor_tensor(out=t[:], in0=acc[:, sub, :], in1=mod[:, 5120:6144], op=ALU.mult), reads=[f"p6_acc{sub}_0", f"p6_acc{sub}_1", "mod"], writes=["p6_t"])
                    em.op("dve", lambda x1t=x1t: nc.vector.scalar_tensor_tensor(out=t[:], in0=x1t[:], scalar=DN_ALPHA, in1=t[:], op0=ALU.mult, op1=ALU.add), reads=[xk, "p6_t"], writes=["p6_t"])
                    self.ln_stats(t, "p6_t", st, mv, rs, "p6_")
                    em.op("act", lambda: nc.scalar.activation(out=xn[:], in_=t[:], func=AF.Identity, scale=rs[:, 0:1], bias=mv[:, 1:2]), reads=["p6_t", "p6_mv", "p6_rs"], writes=["p6_xn"])
                    em.op("dve", lambda: nc.vector.tensor_tensor(out=xn[:], in0=xn[:], in1=l2w[:], op=ALU.mult), reads=["p6_xn", "p6_l2w"], writes=["p6_xn"])
                    em.op("pool", lambda: nc.gpsimd.tensor_tensor(out=ot[:], in0=xn[:], in1=l2b[:], op=ALU.add), reads=["p6_xn", "p6_l2b"], writes=["p6_ot"])
                    drows = slice((n - t0 * 8) * 128, (n - t0 * 8 + 1) * 128)
                    em.dma("sp", dst[drows, :], ot[:], reads=["p6_ot"], writes=["dst%d" % layer], is_output=is_output)

    def build_all(self):
        nc = self.nc
        out = self.make_out()
        with nc.sbuf_tensor("mod", [128, 6 * D], F32) as mod:
            for layer in range(DEPTH):
                x_src = self.I["x"] if layer == 0 else self.I["x2_d"]
                self.phase0(layer, mod)
                self.phase1(layer, x_src, mod)
                self.phase2(layer)
                self.phase3(layer)
                self.phase4(layer)
                self.phase5(layer, x_src, mod)
                self.phase6(layer, mod, moe=(layer % 2 == 1), dst=(out if layer == DEPTH - 1 else self.I["x2_d"]), is_output=(layer == DEPTH - 1))
            self.em.barrier()
            self.em.finish()


def _bf(a):
    return np.asarray(a, dtype=np.float32).astype(ml_dtypes.bfloat16)


def make_consts():
    c = {}
    c["ident"] = _bf(np.eye(128))
    j = np.arange(128)[:, None]
    k = np.arange(S)[None, :]
    c["E_all"] = _bf((k // 64 == j).astype(np.float32))
    cc = np.arange(512)[:, None]
    jj = np.arange(128)[None, :]
    ov = ((cc * 16 < jj * 64 + 64) & (cc * 16 + 32 > jj * 64) & (cc < 511)).astype(np.float32)
    c["overlap"] = _bf(ov)
    cl = np.arange(128)[:, None]
    ql = np.arange(128)[None, :]
    c["cmaskb"] = _bf(np.stack([np.where(16 * cl + 31 <= 128 * pi + ql, 0.0, NEGB) for pi in range(17)]))
    c["cb"] = _bf(np.where(cl <= ql, 0.0, NEGB))
    c["wlo"] = _bf(np.where(cl > ql, 0.0, NEGB))
    bonus = np.zeros((NT, 128, 128), np.float32)
    qq = np.arange(128)[:, None]
    jb = np.arange(128)[None, :]
    for n in range(NT):
        i = 2 * n + (qq >= 64)
        forced = (jb == 0) | (jb == i) | (jb == i - 1)
        bonus[n] = np.where(jb <= i, np.where(forced, 1e4, 0.0), -1e30)
    c["bonus"] = bonus
    c["tri"] = (cl <= ql).astype(np.float32)
    c["tris"] = (cl > ql).astype(np.float32)
    c["ones"] = np.ones((128, 128), np.float32)
    c["tri_bf"] = _bf(c["tri"])
    return c


def arrange_w_in(w_in):
    L = w_in.shape[0]
    o = {}
    off = 0
    for name, sz in (("q", 512), ("kc", 128), ("vc", 128), ("ks", 128), ("vs", 128), ("kw", 128), ("vw", 128), ("g", 24), ("z", 512), ("xbc", 1024), ("dt", 8)):
        o[name] = (off, off + sz)
        off += sz
    cols = []
    for r in range(4):
        for g in range(2):
            h = g * 4 + r
            cols += list(range(o["q"][0] + h * 64, o["q"][0] + (h + 1) * 64))
    cols += list(range(*o["ks"]))
    cols += list(range(*o["kw"]))
    for nm in ("kc", "vc"):
        for g in range(2):
            blk = list(range(o[nm][0] + g * 64, o[nm][0] + (g + 1) * 64))
            cols += blk + blk
    cols += list(range(*o["xbc"]))
    cols += list(range(*o["vs"])) + list(range(*o["vw"])) + list(range(*o["g"])) + list(range(*o["dt"]))
    cols += list(range(*o["z"]))
    assert len(cols) == W1C
    return np.ascontiguousarray(w_in[:, :, np.asarray(cols)])


def host_inputs(inputs):
    f = lambda a: np.ascontiguousarray(np.asarray(a, dtype=np.float32))
    sh = {}
    sh["ada_w"] = f(inputs["ada_w"]); sh["ada_b"] = f(inputs["ada_b"])
    sh["w_in_r"] = arrange_w_in(f(inputs["w_in"]))
    for kv in "kv":
        sh[f"cmp_pos_{kv}"] = np.ascontiguousarray(f(inputs[f"cmp_pos_{kv}"]).reshape(2, 16, 128).transpose(0, 2, 1))
        sh[f"cmp_w1_{kv}"] = f(inputs[f"cmp_w1_{kv}"]); sh[f"cmp_w2_{kv}"] = f(inputs[f"cmp_w2_{kv}"])
    sh["conv_w_r"] = np.ascontiguousarray(f(inputs["conv_w"]).reshape(2, 4, 8, 128).transpose(0, 3, 2, 1))
    sh["conv_b_r"] = np.ascontiguousarray(f(inputs["conv_b"]).reshape(2, 8, 128).transpose(0, 2, 1))
    for n in ("attn_norm_w", "dt_bias", "a_log", "d_skip", "ssm_norm_w", "w_out", "ln1_w", "ln1_b", "ln2_w", "ln2_b",
              "ffn_w_gate", "ffn_w_up", "ffn_w_down"):
        sh[n] = f(inputs[n])
    sh["router_w"] = f(inputs["router_w"])[0]
    sh["exp_w_gate"] = f(inputs["exp_w_gate"])[0]; sh["exp_w_up"] = f(inputs["exp_w_up"])[0]; sh["exp_w_down"] = f(inputs["exp_w_down"])[0]
    sh.update(make_consts())
    x = f(inputs["x"]); c = f(inputs["c"])
    per = []
    for b in range(x.shape[0]):
        per.append({"x": x[b], "cT": np.ascontiguousarray(c[b].reshape(8, 128).T)})
    return sh, per


_CACHE = {}

MODE = "fused"

_SET_A = ["qT_d", "kselT_d", "kwinT_d", "vsel_d", "vwin_d", "gates_d", "kcT_d", "vc_d", "ssm_d"]
SPLIT_N = 40


def _build_launch(idx):
    if idx in (0, 1):
        layer = idx
        ein = ["x2_d"] if layer == 1 else []
        eout = ["x2_d"] if layer == 0 else ["x1_d", "u2T_d"]
        kb = K(ext_in=ein, ext_out=eout)
        kb.declare()
        nc = kb.nc
        with nc.sbuf_tensor("mod", [128, 6 * D], F32) as mod:
            x_src = kb.I["x"] if layer == 0 else kb.I["x2_d"]
            kb.phase0(layer, mod)
            kb.phase1(layer, x_src, mod)
            kb.phase2(layer)
            kb.phase4(layer)
            kb.phase3(layer)
            kb.phase5(layer, x_src, mod)
            if layer == 0:
                kb.phase6(layer, mod, moe=False, dst=kb.I["x2_d"])
            kb.em.barrier(); kb.em.finish()
        return kb, ein, eout
    t0, t1 = {2: (0, 4), 3: (4, 8)}[idx]
    ein = ["x1_d", "u2T_d"]
    oname = "out_%d" % idx
    eout = [oname]
    kb = K(ext_in=ein, ext_out=eout)
    kb.declare()
    kb.stab[oname] = ([(t1 - t0) * 1024, D], F32)
    nc = kb.nc
    with nc.sbuf_tensor("mod", [128, 6 * D], F32) as mod:
        kb.phase0(1, mod)
        kb.phase6(1, mod, moe=True, dst=kb.I[oname], n_tt=t1, t0=t0, is_output=True)
        kb.em.barrier(); kb.em.finish()
    return kb, ein, eout


def _run(kb, sh, per, hand, n_cores):
    names = [n for n in kb.I.keys() if (n in kb.itab or n in kb.ext_in)]
    in_maps = []
    for b in range(n_cores):
        m = {}
        for n in names:
            if n in kb.ext_in:
                m[n] = hand[b][n]
            else:
                m[n] = per[b][n] if n in per[b] else sh[n]
        in_maps.append(m)
    return run_bass_kernel_spmd(kb.nc, in_maps, core_ids=list(range(n_cores)))


def kernel(**inputs):
    sh, per = host_inputs(inputs)
    nb = len(per)
    if MODE == "fused":
        if "kb" not in _CACHE:
            kb = K()
            kb.declare()
            kb.build_all()
            _CACHE["kb"] = kb
        kb = _CACHE["kb"]
        res = _run(kb, sh, per, None, nb)
        return np.stack([np.asarray(r["out"], dtype=np.float32) for r in res.results], axis=0)
    hand = [dict() for _ in range(nb)]
    for idx in range(4):
        key = "L%d" % idx
        if key not in _CACHE:
            _CACHE[key] = _build_launch(idx)
        kb, ein, eout = _CACHE[key]
        res = _run(kb, sh, per, hand, nb)
        for b in range(nb):
            for n in eout:
                hand[b][n] = res.results[b][n]
    outs = []
    for b in range(nb):
        outs.append(np.concatenate([np.asarray(hand[b]["out_%d" % i], dtype=np.float32) for i in (2, 3)], axis=0))
    return np.stack(outs, axis=0)
```
